# Optimizing a Trainium2 kernel written in Bass

```python
import jax, jax.numpy as jnp
from jax import lax
import numpy as np

D_MODEL = 2048
BATCH = 2
SEQ = 4096
DEPTH = 1

CONV_WIDTH = 1024
CONV_K = 3
N_HEADS = 8
HEAD_DIM = 128
ATTN_WIDTH = N_HEADS * HEAD_DIM
ROT_DIM = HEAD_DIM // 4
ROPE_THETA = 500000.0
MOBA_BLOCK = 256
MOBA_TOPK = 3
Q_CHUNK = 32
PEER_HEADS = 8
PEER_NKEYS = 128
PEER_N_EXPERTS = PEER_NKEYS * PEER_NKEYS
PEER_KEY_DIM = 256
PEER_HALF = PEER_KEY_DIM // 2
PEER_TOPK = 16
TOK_CHUNK = 128
RMS_EPS = 1e-6
IN_SIZES = (CONV_WIDTH, CONV_WIDTH, CONV_WIDTH, ATTN_WIDTH, ATTN_WIDTH, ATTN_WIDTH, D_MODEL, D_MODEL)
IN_WIDTH = sum(IN_SIZES)
IN_SPLITS = tuple(int(s) for s in np.cumsum(IN_SIZES)[:-1])

kernel_name = "hybrid_conv_moba_peer_block"


def rms_norm(x, g):
    x32 = x.astype(jnp.float32)
    y = x32 * lax.rsqrt(jnp.mean(x32 * x32, axis=-1, keepdims=True) + RMS_EPS)
    return y.astype(x.dtype) * g


def partial_rotary(x, positions):
    half = ROT_DIM // 2
    inv_freq = 1.0 / (ROPE_THETA ** (jnp.arange(0, ROT_DIM, 2, dtype=jnp.float32) / ROT_DIM))
    ang = positions.astype(jnp.float32)[:, :, None] * inv_freq
    cos = jnp.cos(ang)[:, :, None, :]
    sin = jnp.sin(ang)[:, :, None, :]
    x32 = x.astype(jnp.float32)
    x1 = x32[..., :half]
    x2 = x32[..., half:ROT_DIM]
    rot = jnp.concatenate([x1 * cos - x2 * sin, x2 * cos + x1 * sin], axis=-1)
    return jnp.concatenate([rot.astype(x.dtype), x[..., ROT_DIM:]], axis=-1)


def short_conv_mixer(b_gate, c_gate, xc, conv_w):
    z = c_gate * xc
    s = z.shape[1]
    zp = jnp.pad(z, ((0, 0), (CONV_K - 1, 0), (0, 0)))
    conv = sum(conv_w[j] * zp[:, j:j + s] for j in range(CONV_K))
    return b_gate * conv


def moba_attention(q, k, v):
    b, s, h, hd = q.shape
    bh = b * h
    nb = -(-s // MOBA_BLOCK)
    s_pad = nb * MOBA_BLOCK
    n_sel = min(MOBA_TOPK, nb)
    scale = hd ** -0.5
    qh = q.transpose(0, 2, 1, 3).reshape(bh, s, hd)
    kp = jnp.pad(k.transpose(0, 2, 1, 3).reshape(bh, s, hd), ((0, 0), (0, s_pad - s), (0, 0)))
    vp = jnp.pad(v.transpose(0, 2, 1, 3).reshape(bh, s, hd), ((0, 0), (0, s_pad - s), (0, 0)))
    kb = kp.reshape(bh, nb, MOBA_BLOCK, hd)
    vb = vp.reshape(bh, nb, MOBA_BLOCK, hd)
    kmean = jnp.mean(kb.astype(jnp.float32), axis=2)

    def one_chunk(c):
        q0 = c * Q_CHUNK
        blk = q0 // MOBA_BLOCK
        qc = lax.dynamic_slice_in_dim(qh, q0, Q_CHUNK, axis=1)
        gate = jnp.einsum('nqd,nbd->nqb', qc.astype(jnp.float32), kmean)
        gate = jnp.where(jnp.arange(nb)[None, None, :] < blk, gate, -jnp.inf)
        _, top_i = lax.top_k(gate, n_sel)
        valid = jnp.arange(n_sel) < blk
        k_sel = jax.vmap(lambda kbn, idx: kbn[idx])(kb, top_i)
        v_sel = jax.vmap(lambda vbn, idx: vbn[idx])(vb, top_i)
        s_sel = jnp.einsum('nqd,nqjkd->nqjk', qc, k_sel).astype(jnp.float32) * scale
        s_sel = jnp.where(valid[None, None, :, None], s_sel, -jnp.inf)
        s_sel = s_sel.reshape(bh, Q_CHUNK, n_sel * MOBA_BLOCK)
        k_own = lax.dynamic_slice_in_dim(kp, blk * MOBA_BLOCK, MOBA_BLOCK, axis=1)
        v_own = lax.dynamic_slice_in_dim(vp, blk * MOBA_BLOCK, MOBA_BLOCK, axis=1)
        s_own = jnp.einsum('nqd,nkd->nqk', qc, k_own).astype(jnp.float32) * scale
        q_pos = q0 + jnp.arange(Q_CHUNK)
        k_pos = blk * MOBA_BLOCK + jnp.arange(MOBA_BLOCK)
        s_own = jnp.where(k_pos[None, None, :] <= q_pos[None, :, None], s_own, -jnp.inf)
        p = jax.nn.softmax(jnp.concatenate([s_sel, s_own], axis=-1), axis=-1)
        p_sel = p[..., :n_sel * MOBA_BLOCK].reshape(bh, Q_CHUNK, n_sel, MOBA_BLOCK).astype(v.dtype)
        p_own = p[..., n_sel * MOBA_BLOCK:].astype(v.dtype)
        return (jnp.einsum('nqjk,nqjkd->nqd', p_sel, v_sel)
                + jnp.einsum('nqk,nkd->nqd', p_own, v_own))

    out = lax.map(one_chunk, jnp.arange(s // Q_CHUNK))
    out = out.transpose(1, 0, 2, 3).reshape(b, h, s, hd)
    return out.transpose(0, 2, 1, 3).reshape(b, s, h * hd)


def peer_ffn(xn, w_pq, sub_keys, expert_u, expert_v):
    b, s, d = xn.shape
    t = b * s
    xt = xn.reshape(t, d)
    qp = (xt @ w_pq).reshape(t, PEER_HEADS, 2, PEER_HALF)
    sc = jnp.einsum('thpd,hpnd->thpn', qp, sub_keys).astype(jnp.float32)
    s_top, i_top = lax.top_k(sc, PEER_TOPK)
    cand = s_top[:, :, 0, :, None] + s_top[:, :, 1, None, :]
    cand_idx = i_top[:, :, 0, :, None] * PEER_NKEYS + i_top[:, :, 1, None, :]
    best_s, best_pos = lax.top_k(cand.reshape(t, PEER_HEADS, PEER_TOPK * PEER_TOPK), PEER_TOPK)
    experts = jnp.take_along_axis(cand_idx.reshape(t, PEER_HEADS, PEER_TOPK * PEER_TOPK), best_pos, axis=-1)
    gates = jax.nn.softmax(best_s, axis=-1)
    n_chunks = t // TOK_CHUNK

    def one_chunk(args):
        xc, idx, g = args
        u = expert_u[idx]
        v = expert_v[idx]
        a = jnp.einsum('thkd,td->thk', u, xc)
        act = (jax.nn.gelu(a.astype(jnp.float32)) * g).astype(xc.dtype)
        return jnp.einsum('thk,thkd->td', act, v)

    y = lax.map(one_chunk, (xt.reshape(n_chunks, TOK_CHUNK, d),
                            experts.reshape(n_chunks, TOK_CHUNK, PEER_HEADS, PEER_TOPK),
                            gates.reshape(n_chunks, TOK_CHUNK, PEER_HEADS, PEER_TOPK)))
    return y.reshape(b, s, d)


def setup_inputs(seed: int = 0) -> dict:
    key = jax.random.key(seed)
    ks = jax.random.split(key, 16)
    f32 = jnp.float32
    nrm = lambda k, shape, sc: jax.random.normal(k, shape, f32) * sc
    x = nrm(ks[0], (BATCH, SEQ, D_MODEL), 1.0)
    offset = jax.random.randint(ks[1], (BATCH, 1), 0, 4096, dtype=jnp.int32)
    positions = offset + jnp.arange(SEQ, dtype=jnp.int32)[None, :]
    return {
        "x": x,
        "positions": positions,
        "attn_norm_g": 1.0 + nrm(ks[2], (DEPTH, D_MODEL), 0.02),
        "w_in": nrm(ks[3], (DEPTH, D_MODEL, IN_WIDTH), D_MODEL ** -0.5),
        "gate_bias": nrm(ks[4], (DEPTH, 2 * D_MODEL), 0.01),
        "conv_w": nrm(ks[5], (DEPTH, CONV_K, CONV_WIDTH), CONV_K ** -0.5),
        "w_branch_conv": nrm(ks[6], (DEPTH, CONV_WIDTH, D_MODEL), CONV_WIDTH ** -0.5),
        "w_branch_attn": nrm(ks[7], (DEPTH, ATTN_WIDTH, D_MODEL), ATTN_WIDTH ** -0.5),
        "w_out": nrm(ks[8], (DEPTH, D_MODEL, D_MODEL), D_MODEL ** -0.5),
        "ffn_norm_g": 1.0 + nrm(ks[9], (DEPTH, D_MODEL), 0.02),
        "w_peer_query": nrm(ks[10], (DEPTH, D_MODEL, PEER_HEADS * PEER_KEY_DIM), D_MODEL ** -0.5),
        "peer_sub_keys": nrm(ks[11], (DEPTH, PEER_HEADS, 2, PEER_NKEYS, PEER_HALF), PEER_HALF ** -0.5),
        "peer_u": nrm(ks[12], (DEPTH, PEER_N_EXPERTS, D_MODEL), D_MODEL ** -0.5),
        "peer_v": nrm(ks[13], (DEPTH, PEER_N_EXPERTS, D_MODEL), PEER_HEADS ** -0.5),
        "final_norm_g": 1.0 + nrm(ks[14], (D_MODEL,), 0.02),
    }


def reference(x, positions, attn_norm_g, w_in, gate_bias, conv_w, w_branch_conv, w_branch_attn,
              w_out, ffn_norm_g, w_peer_query, peer_sub_keys, peer_u, peer_v, final_norm_g):
    b, s, _ = x.shape
    for l in range(DEPTH):
        h = rms_norm(x, attn_norm_g[l])
        proj = h @ w_in[l]
        cb, cc, cx, q, k, v, g_conv, g_attn = jnp.split(proj, IN_SPLITS, axis=-1)
        y_conv = short_conv_mixer(cb, cc, cx, conv_w[l]) @ w_branch_conv[l]
        q = partial_rotary(q.reshape(b, s, N_HEADS, HEAD_DIM), positions)
        k = partial_rotary(k.reshape(b, s, N_HEADS, HEAD_DIM), positions)
        v = v.reshape(b, s, N_HEADS, HEAD_DIM)
        y_attn = moba_attention(q, k, v) @ w_branch_attn[l]
        gb = gate_bias[l]
        gate_a = jax.nn.sigmoid(g_conv + gb[:D_MODEL])
        gate_b = jax.nn.sigmoid(g_attn + gb[D_MODEL:])
        x = x + (gate_a * y_conv + gate_b * y_attn) @ w_out[l]
        x = x + peer_ffn(rms_norm(x, ffn_norm_g[l]), w_peer_query[l], peer_sub_keys[l], peer_u[l], peer_v[l])
    return rms_norm(x, final_norm_g)
```

```python
import os
from contextlib import ExitStack

import numpy as np
import concourse.bass as bass
import concourse.mybir as mybir
from concourse.bass_utils import run_bass_kernel_spmd

F32 = mybir.dt.float32
F32R = mybir.dt.float32r
BF16 = mybir.dt.bfloat16
I32 = mybir.dt.int32
U32 = mybir.dt.uint32
ALU = mybir.AluOpType
AF = mybir.ActivationFunctionType
AX = mybir.AxisListType

ENGS = ("pe", "act", "dve", "pool", "sp")
BLK = {"pe": "tensor", "act": "scalar", "dve": "vector", "pool": "gpsimd", "sp": "sync"}

D = 2048
NT = 1024
SEQ = 4096
INW = 10240
EPS = 1e-6
MAGIC = 12582912.0
NEG = -1.0e30


class Prog:
    def __init__(self, nc, es, n_dma_sems=48):
        self.nc = nc
        self.ops = {e: [] for e in ENGS}
        self.esem = {e: es.enter_context(nc.semaphore("s_" + e)) for e in ENGS}
        self.ecnt = {e: 0 for e in ENGS}
        self.dsem = [es.enter_context(nc.semaphore("d%d" % i)) for i in range(n_dma_sems)]
        self.dcnt = [0] * n_dma_sems
        self.dnext = 0
        self.waited = {e: {} for e in ENGS}
        self.lastw = {}
        self.readers = {}
        self.block = None
        self.mute = False

    def _sem(self, k):
        return self.esem[k] if isinstance(k, str) else self.dsem[k]

    def _need(self, e, ticket, waits):
        if ticket is None:
            return
        k, v = ticket
        if k == e and e == "pe":
            return
        if self.waited[e].get(k, 0) >= v:
            return
        if waits.get(k, 0) < v:
            waits[k] = v

    def op(self, e, fn, reads=(), writes=(), dma=False):
        if self.mute:
            return None
        waits = {}
        for r in reads:
            self._need(e, self.lastw.get(r), waits)
        for w in writes:
            self._need(e, self.lastw.get(w), waits)
            for t in self.readers.get(w, ()):
                self._need(e, t, waits)
        if dma:
            i = self.dnext
            self.dnext = (self.dnext + 1) % len(self.dsem)
            if self.dcnt[i] > 0:
                self._need(e, (i, self.dcnt[i]), waits)
            self.dcnt[i] += 16
            ticket = (i, self.dcnt[i])
        else:
            self.ecnt[e] += 1
            ticket = (e, self.ecnt[e])
        for k, v in waits.items():
            self.waited[e][k] = v
        self.ops[e].append((list(waits.items()), fn, ticket, dma))
        for r in reads:
            self.readers.setdefault(r, []).append(ticket)
        for w in writes:
            self.lastw[w] = ticket
            self.readers[w] = []
        return ticket

    def dma(self, e, out, in_, reads=(), writes=()):
        kw = {"max_dma_last_dim": 4096} if e == "pool" else {}
        return self.op(e, lambda g: g.dma_start(out=out, in_=in_, **kw), reads, writes, dma=True)

    def barrier(self):
        for e in ENGS:
            waits = {}
            for k in ENGS:
                if self.ecnt[k] > 0:
                    self._need(e, (k, self.ecnt[k]), waits)
            for i, c in enumerate(self.dcnt):
                if c > 0:
                    self._need(e, (i, c), waits)
            for k, v in waits.items():
                self.waited[e][k] = v
            if waits:
                self.ops[e].append((list(waits.items()), None, None, False))
        self.lastw = {}
        self.readers = {}

    def flush(self):
        for e in ENGS:
            lst = self.ops[e]
            if not lst:
                continue
            self.ops[e] = []

            def body(eng, lst=lst):
                for waits, fn, ticket, dma in lst:
                    for k, v in waits:
                        eng.wait_ge(self._sem(k), v)
                    if fn is None:
                        continue
                    ins = fn(eng)
                    k, v = ticket
                    ins.then_inc(self._sem(k), 16 if dma else 1)

            getattr(self.block, BLK[e])(body)


def fv(ap):
    return ap.bitcast(F32)


def build_nc(dbg=False, phases="ABCD012"):
    nc = bass.Bass("TRN2", target_bir_lowering=False)

    def din(name, shape, dt=F32):
        return nc.dram_tensor(name, list(shape), dt, kind="ExternalInput").ap()

    skind = "ExternalOutput" if dbg else "Internal"

    def dscr(name, shape):
        return nc.dram_tensor(name, list(shape), F32, kind=skind).ap()

    xrot_d = din("xrot", [SEQ, D])
    xhalo_d = din("xhalo", [128, D])
    posb_d = din("posb", [128, SEQ], I32)
    pastm_d = din("pastmask", [128, 8, 16])
    ownm_d = din("ownmask", [128, 8, 16])
    ident_d = din("ident", [128, 128])
    rmat_d = din("rmat", [128, 128])
    invf_d = din("invf2", [128, 1])
    iota_d = din("iota128", [128, 128])
    g1_d = din("g1bc", [128, D])
    g2_d = din("g2bc", [128, D])
    g3_d = din("g3bc", [128, D])
    gbias_d = din("gbias", [128, 32])
    convw_d = din("convw", [128, 8, 3])
    skT_d = din("skT", [128, 16, 128])
    w_qkv_l = din("w_qkv_l", [6, 128, 16, 512])
    w_cv_l = din("w_cv_l", [8, 128, 16, 3, 128])
    w_gt_l = din("w_gt_l", [16, 128, 16, 2, 128])
    w_br_l = din("w_br_l", [16, 128, 2, 8, 128])
    w_out_l = din("w_out_l", [8, 128, 16, 256])
    w_pq_l = din("w_pq_l", [16, 128, 16, 128])
    u_d = din("u_l", [128, 128, 16, 128])
    v_d = din("v_l", [128, 128, D])
    out_d = nc.dram_tensor("out", [NT, D], F32, kind="ExternalOutput").ap()

    kT_s = dscr("kT_s", [8, 128, SEQ])
    qT_s = dscr("qT_s", [8, 128, NT])
    v_s = dscr("v_s", [SEQ, 1024])
    att_s = dscr("att_s", [NT, 1024])
    x2_s = dscr("x2_s", [NT, D])
    qp_s = nc.dram_tensor("qp_s", [16, 128, NT], F32, kind="Internal").ap()
    Wd = nc.dram_tensor("Wd_s", [128, 128, NT], BF16, kind="Internal").ap()

    QSCALE = 128.0 ** -0.5

    with ExitStack() as es:
        p = Prog(nc, es)

        tcount = [0]

        def T(st, name, shape, dt=F32):
            tcount[0] += 1
            return st.enter_context(nc.sbuf_tensor("%s_%d" % (name, tcount[0]), list(shape), dt))

        ps = [es.enter_context(nc.psum_tensor("psb%d" % i, [128, 512], F32)) for i in range(8)]
        PK = ["ps%d" % i for i in range(8)]
        ident = T(es, "ident", [128, 128])
        iota = T(es, "iota", [128, 128])

        with nc.Block() as block:
            p.block = block
            p.dma("sp", ident[:], ident_d, writes=["ident"])
            p.dma("sp", iota[:], iota_d, writes=["iota"])

            def norm_tile(src_rows, gbc, gkey, xt, xsc, small, dstT, dkey, col0, pst, tagn):
                p.dma("sp", xt[:], src_rows, writes=["xt" + tagn])
                p.op("act", lambda g: g.activation(out=xsc[:], in_=xt[:], func=AF.Square, accum_out=small[:, 0:1]),
                     reads=["xt" + tagn], writes=["xsc" + tagn, "sm" + tagn])
                p.op("act", lambda g: g.activation(out=small[:, 1:2], in_=small[:, 0:1], func=AF.Sqrt, scale=1.0 / D, bias=EPS),
                     reads=["sm" + tagn], writes=["sm" + tagn])
                p.op("dve", lambda g: g.reciprocal(out=small[:, 2:3], in_=small[:, 1:2]), reads=["sm" + tagn], writes=["sm" + tagn])
                p.op("dve", lambda g: g.scalar_tensor_tensor(out=xsc[:], in0=xt[:], scalar=small[:, 2:3], in1=gbc[:],
                                                              op0=ALU.mult, op1=ALU.mult),
                     reads=["xt" + tagn, "sm" + tagn, gkey], writes=["xsc" + tagn])
                for g4 in range(4):
                    b = pst[g4 % 2]
                    for i in range(4):
                        dc = g4 * 4 + i
                        p.op("pe", lambda g, b=b, i=i, dc=dc: g.transpose(ps[b][:, i * 128:(i + 1) * 128], xsc[:, dc * 128:(dc + 1) * 128], ident[:]),
                             reads=["xsc" + tagn, "ident"], writes=[PK[b]])
                    p.op("act", lambda g, b=b, g4=g4: g.copy(out=dstT[:, g4 * 4:(g4 + 1) * 4, col0:col0 + 128],
                                                             in_=ps[b][:].rearrange("p (a b) -> p a b", b=128)),
                         reads=[PK[b]], writes=[dkey])

            if "A" in phases:
                with ExitStack() as sa:
                    g1bc = T(sa, "g1bc", [128, D])
                    posb = T(sa, "posb", [128, 512], I32)
                    invf = T(sa, "invf", [128, 1])
                    rmat = T(sa, "rmat", [128, 128], F32R)
                    xt = T(sa, "xtA", [128, D]); xsc = T(sa, "xscA", [128, D]); small = T(sa, "smallA", [128, 4])
                    xnT = [T(sa, "xnTA%d" % i, [128, 16, 512], F32R) for i in range(2)]
                    wb = [T(sa, "wbA%d" % i, [128, 16, 512], F32R) for i in range(2)]
                    cs = [T(sa, "csA%d" % i, [128, 2, 512]) for i in range(2)]
                    tmp = [T(sa, "tmpA%d" % i, [128, 512]) for i in range(4)]
                    ksb = [T(sa, "ksbA%d" % i, [128, 512], F32R) for i in range(2)]
                    t1 = [T(sa, "t1A%d" % i, [128, 512]) for i in range(2)]
                    t2 = [T(sa, "t2A%d" % i, [128, 512]) for i in range(2)]
                    kst = [T(sa, "kstA%d" % i, [128, 512]) for i in range(2)]
                    vst = [T(sa, "vstA%d" % i, [128, 512]) for i in range(2)]
                    p.dma("sp", g1bc[:], g1_d, writes=["g1bc"])
                    p.dma("sp", invf[:], invf_d, writes=["invf"])
                    p.dma("pool", rmat[:], rmat_d, writes=["rmat"])

                    wcount = [0]
                    kcount = [0]
                    vcount = [0]

                    def blocks_of(ck):
                        bl = [("k", i) for i in range(2)] + [("v", i) for i in range(2)]
                        if ck < 2:
                            bl += [("q", i) for i in range(2)]
                        return bl

                    allblocks = [(ck, kind, bi) for ck in range(8) for (kind, bi) in blocks_of(ck)]

                    def load_w(idx):
                        ck, kind, bi = allblocks[idx]
                        blk = {"q": 0, "k": 2, "v": 4}[kind] + bi
                        b = idx % 2
                        p.dma("pool", wb[b][:], w_qkv_l[blk], writes=["wb%d" % b])

                    def prep(ck):
                        xb = xnT[ck % 2]
                        xk = "xnT%d" % (ck % 2)
                        tok0 = ck * 512
                        for tt in range(4):
                            ti = ck * 4 + tt
                            norm_tile(xrot_d[ti * 128:(ti + 1) * 128, :], g1bc, "g1bc", xt, xsc, small, xb, xk, tt * 128, (0, 1), "A")
                            yield
                        c = cs[ck % 2]
                        ck_ = "cs%d" % (ck % 2)
                        y, r_, f_, yc = tmp
                        p.dma("sp", posb[:], posb_d[:, tok0:tok0 + 512], writes=["posb"])
                        p.op("dve", lambda g: g.tensor_copy(out=y[:], in_=posb[:]), reads=["posb"], writes=["tmp0"])
                        p.op("dve", lambda g: g.tensor_scalar(out=y[:], in0=y[:], scalar1=invf[:, 0:1], scalar2=None, op0=ALU.mult),
                             reads=["tmp0", "invf"], writes=["tmp0"])
                        p.op("dve", lambda g: g.tensor_scalar(out=r_[:], in0=y[:], scalar1=MAGIC, scalar2=None, op0=ALU.add), reads=["tmp0"], writes=["tmp1"])
                        p.op("dve", lambda g: g.tensor_scalar(out=r_[:], in0=r_[:], scalar1=-MAGIC, scalar2=None, op0=ALU.add), reads=["tmp1"], writes=["tmp1"])
                        p.op("dve", lambda g: g.tensor_tensor(out=f_[:], in0=y[:], in1=r_[:], op=ALU.subtract), reads=["tmp0", "tmp1"], writes=["tmp2"])
                        p.op("act", lambda g, c=c: g.activation(out=c[:, 1, :], in_=f_[:], func=AF.Sin, scale=6.283185),
                             reads=["tmp2"], writes=[ck_])
                        p.op("dve", lambda g: g.tensor_scalar(out=yc[:], in0=y[:], scalar1=0.25, scalar2=None, op0=ALU.add), reads=["tmp0"], writes=["tmp3"])
                        p.op("dve", lambda g: g.tensor_scalar(out=r_[:], in0=yc[:], scalar1=MAGIC, scalar2=None, op0=ALU.add), reads=["tmp3"], writes=["tmp1"])
                        p.op("dve", lambda g: g.tensor_scalar(out=r_[:], in0=r_[:], scalar1=-MAGIC, scalar2=None, op0=ALU.add), reads=["tmp1"], writes=["tmp1"])
                        p.op("dve", lambda g: g.tensor_tensor(out=f_[:], in0=yc[:], in1=r_[:], op=ALU.subtract), reads=["tmp3", "tmp1"], writes=["tmp2"])
                        p.op("act", lambda g, c=c: g.activation(out=c[:, 0, :], in_=f_[:], func=AF.Sin, scale=6.283185),
                             reads=["tmp2"], writes=[ck_])
                        yield

                    pending = []

                    def flush_pending():
                        while pending:
                            pending.pop(0)()

                    def make_tail(a, c, ck_, kind, h, tok0):
                        def tail():
                            pb2 = 4 + a
                            p.op("pe", lambda g, a=a, pb2=pb2: g.matmul(ps[pb2][:], rmat[:], ksb[a][:], start=True, stop=True),
                                 reads=["rmat", "ksb%d" % a], writes=[PK[pb2]])
                            p.op("dve", lambda g, a=a, c=c: g.tensor_tensor(out=t1[a][:], in0=fv(ksb[a][:]), in1=c[:, 0, :], op=ALU.mult),
                                 reads=["ksb%d" % a, ck_], writes=["t1%d" % a])
                            p.op("dve", lambda g, a=a, c=c, pb2=pb2: g.tensor_tensor(out=t2[a][:], in0=ps[pb2][:], in1=c[:, 1, :], op=ALU.mult),
                                 reads=[PK[pb2], ck_], writes=["t2%d" % a])
                            p.op("pool", lambda g, a=a: g.tensor_tensor(out=kst[a][:], in0=t1[a][:], in1=t2[a][:], op=ALU.add),
                                 reads=["t1%d" % a, "t2%d" % a], writes=["kst%d" % a])
                            if kind == "k":
                                p.dma("sp", kT_s[h, :, tok0:tok0 + 512], kst[a][:], reads=["kst%d" % a])
                            else:
                                p.dma("sp", qT_s[h, :, tok0:tok0 + 512], kst[a][:], reads=["kst%d" % a])
                        return tail

                    for _ in prep(0):
                        pass
                    load_w(0)
                    nxt = None
                    for idx, (ck, kind, bi) in enumerate(allblocks):
                        xb = xnT[ck % 2]
                        xk = "xnT%d" % (ck % 2)
                        tok0 = ck * 512
                        if (kind, bi) == ("k", 0):
                            flush_pending()
                            nxt = prep(ck + 1) if ck + 1 < 8 else None
                        if idx + 1 < len(allblocks):
                            load_w(idx + 1)
                        b = idx % 2
                        wk = "wb%d" % b
                        c = cs[ck % 2]
                        ck_ = "cs%d" % (ck % 2)
                        if kind in ("k", "q"):
                            for hh in range(4):
                                h = bi * 4 + hh
                                a = kcount[0] % 2
                                kcount[0] += 1
                                pb = 2 + a
                                for dc in range(16):
                                    p.op("pe", lambda g, pb=pb, b=b, dc=dc, hh=hh, xb=xb: g.matmul(ps[pb][:], wb[b][:, dc, hh * 128:(hh + 1) * 128], xb[:, dc, :],
                                                                                                   start=(dc == 0), stop=(dc == 15)),
                                         reads=[wk, xk], writes=[PK[pb]])
                                flush_pending()
                                sc_ = QSCALE if kind == "q" else 1.0
                                p.op("act", lambda g, a=a, pb=pb, sc_=sc_: g.activation(out=ksb[a][:], in_=ps[pb][:], func=AF.Copy, scale=sc_),
                                     reads=[PK[pb]], writes=["ksb%d" % a])
                                pending.append(make_tail(a, c, ck_, kind, h, tok0))
                        else:
                            for tt in range(4):
                                a = vcount[0] % 2
                                vcount[0] += 1
                                pb = 6 + a
                                for dc in range(16):
                                    p.op("pe", lambda g, pb=pb, b=b, dc=dc, tt=tt, xb=xb: g.matmul(ps[pb][:], xb[:, dc, tt * 128:(tt + 1) * 128], wb[b][:, dc, :],
                                                                                                   start=(dc == 0), stop=(dc == 15)),
                                         reads=[wk, xk], writes=[PK[pb]])
                                if tt == 0:
                                    flush_pending()
                                p.op("act", lambda g, a=a, pb=pb: g.copy(out=vst[a][:], in_=ps[pb][:]), reads=[PK[pb]], writes=["vst%d" % a])
                                r0 = ck * 512 + tt * 128
                                p.dma("sp", v_s[r0:r0 + 128, bi * 512:(bi + 1) * 512], vst[a][:], reads=["vst%d" % a])
                        for _ in range(2):
                            if nxt is not None:
                                try:
                                    next(nxt)
                                except StopIteration:
                                    nxt = None
                    flush_pending()
                    if nxt is not None:
                        for _ in nxt:
                            pass
                    p.barrier()
                    p.flush()

            if "B" in phases:
                with ExitStack() as sb:
                    pastm = T(sb, "pastm", [128, 8, 16]); ownm = T(sb, "ownm", [128, 8, 16])
                    KT = [T(sb, "KT%d" % i, [128, SEQ], F32R) for i in range(2)]
                    VA = [T(sb, "VA%d" % i, [128, 32, 130], BF16) for i in range(2)]
                    QT = [T(sb, "QT%d" % i, [128, NT], F32R) for i in range(2)]
                    km = T(sb, "km", [128, 16]); kmr = T(sb, "kmr", [128, 16], F32R)
                    gm = T(sb, "gm", [128, 16]); m8 = T(sb, "m8", [128, 8]); thr = T(sb, "thr", [128, 1])
                    sel2 = [T(sb, "sel%d" % i, [128, 8, 16]) for i in range(2)]
                    pT = [T(sb, "pT%d" % i, [128, 512], BF16) for i in range(4)]
                    pM = [T(sb, "pM%d" % i, [128, 512], BF16) for i in range(4)]
                    acc = T(sb, "acc", [128, 4, 130]); rec = T(sb, "rec", [128, 4])
                    tmpm = [T(sb, "tmpm%d" % i, [128, 4, 130]) for i in range(2)]
                    ao = [T(sb, "ao%d" % i, [128, 4, 128]) for i in range(2)]
                    ones2 = T(sb, "ones2", [128, 32, 2])
                    p.dma("sp", pastm[:], pastm_d, writes=["pastm"])
                    p.dma("sp", ownm[:], ownm_d, writes=["ownm"])
                    p.op("pool", lambda g: g.memset(ones2[:, :, 0:1], 1.0), writes=["ones2"])
                    p.op("pool", lambda g: g.memset(ones2[:, :, 1:2], 0.0), writes=["ones2"])
                    for i in range(2):
                        p.op("pool", lambda g, i=i: g.tensor_copy(out=VA[i][:, :, 128:130], in_=ones2[:]), reads=["ones2"], writes=["VA%d" % i])

                    v_hv = v_s.rearrange("(n p) (h d) -> h p n d", p=128, d=128)

                    def load_head(h):
                        b = h % 2
                        p.dma("pool", KT[b][:], kT_s[h], writes=["KT%d" % b])
                        p.dma("pool", VA[b][:, :, 0:128], v_hv[h], writes=["VA%d" % b])
                        p.dma("pool", QT[b][:], qT_s[h], writes=["QT%d" % b])

                    def head_prologue(h):
                        b = h % 2
                        sel = sel2[b]
                        selk = "sel%d" % b
                        kk, qk = "KT%d" % b, "QT%d" % b
                        p.op("dve", lambda g, b=b: g.tensor_reduce(out=km[:], in_=fv(KT[b][:]).rearrange("p (n k) -> p n k", k=256), axis=AX.X, op=ALU.add),
                             reads=[kk], writes=["km"])
                        p.op("act", lambda g: g.activation(out=kmr[:], in_=km[:], func=AF.Copy, scale=1.0 / 256.0), reads=["km"], writes=["kmr"])
                        for ti in range(8):
                            p.op("pe", lambda g, b=b, ti=ti: g.matmul(ps[0][:, 0:16], QT[b][:, ti * 128:(ti + 1) * 128], kmr[:], start=True, stop=True),
                                 reads=[qk, "kmr"], writes=[PK[0]])
                            p.op("dve", lambda g, ti=ti: g.tensor_tensor(out=gm[:], in0=ps[0][:, 0:16], in1=pastm[:, ti, :], op=ALU.add),
                                 reads=[PK[0], "pastm"], writes=["gm"])
                            p.op("dve", lambda g: g.max(out=m8[:], in_=gm[:]), reads=["gm"], writes=["m8"])
                            p.op("dve", lambda g: g.tensor_scalar(out=thr[:], in0=m8[:, 2:3], scalar1=-1.0e29, scalar2=None, op0=ALU.max),
                                 reads=["m8"], writes=["thr"])
                            p.op("dve", lambda g, ti=ti, sel=sel: g.tensor_scalar(out=sel[:, ti, :], in0=gm[:], scalar1=thr[:, 0:1], scalar2=None, op0=ALU.is_ge),
                                 reads=["gm", "thr"], writes=[selk])
                            p.op("dve", lambda g, ti=ti, sel=sel: g.tensor_tensor(out=sel[:, ti, :], in0=sel[:, ti, :], in1=ownm[:, ti, :], op=ALU.max),
                                 reads=[selk, "ownm"], writes=[selk])

                    steps = []
                    for h in range(8):
                        for qi in range(2):
                            jbs = [jb for jb in range(16) if not (jb < 4 and jb > 2 * qi + 1)]
                            for jb in jbs:
                                for kt_i in range(2):
                                    steps.append(dict(h=h, qi=qi, jb=jb, kt_i=kt_i, kt=2 * jb + kt_i,
                                                      first=(jb == jbs[0] and kt_i == 0), last=(jb == jbs[-1] and kt_i == 1)))
                    LA = 2
                    srcs = {}

                    def emit_score(n):
                        s = steps[n]
                        h, qi, jb, kt = s["h"], s["qi"], s["jb"], s["kt"]
                        b = h % 2
                        if s["first"] and qi == 0:
                            head_prologue(h)
                        kk, qk = "KT%d" % b, "QT%d" % b
                        sb_ = 1 + (n % 3)
                        pt = pT[n % 4]
                        ptk = "pT%d" % (n % 4)
                        p.op("pe", lambda g, b=b, kt=kt, qi=qi, sb_=sb_: g.matmul(ps[sb_][:], KT[b][:, kt * 128:(kt + 1) * 128], QT[b][:, qi * 512:(qi + 1) * 512],
                                                                                start=True, stop=True),
                             reads=[kk, qk], writes=[PK[sb_]])
                        p.op("act", lambda g, pt=pt, sb_=sb_: g.activation(out=pt[:], in_=ps[sb_][:], func=AF.Exp), reads=[PK[sb_]], writes=[ptk])
                        src_, srck = pt, ptk
                        if jb < 4:
                            base = qi * 512 - kt * 128
                            if base - 127 < 0:
                                pm = pM[n % 4]
                                pmk = "pM%d" % (n % 4)
                                p.op("pool", lambda g, pm=pm, pt=pt, base=base: g.affine_select(out=pm[:], in_=pt[:], pattern=[[1, 512]], compare_op=ALU.is_ge,
                                                                                              fill=0.0, base=base, channel_multiplier=-1),
                                     reads=[ptk], writes=[pmk])
                                src_, srck = pm, pmk
                        srcs[n] = (src_, srck)

                    def emit_pv(n):
                        s = steps[n]
                        h, qi, jb, kt, kt_i = s["h"], s["qi"], s["jb"], s["kt"], s["kt_i"]
                        b = h % 2
                        sel = sel2[b]
                        selk = "sel%d" % b
                        vk = "VA%d" % b
                        src_, srck = srcs.pop(n)
                        if s["first"]:
                            p.op("pool", lambda g: g.memset(acc[:], 0.0), writes=["acc"])
                        pvb = 4 + ((n // 2) % 2) * 2
                        for qs in range(4):
                            bank = pvb + qs // 2
                            o0 = (qs % 2) * 130
                            p.op("pe", lambda g, src_=src_, qs=qs, bank=bank, o0=o0, kt=kt, kt_i=kt_i, b=b: g.matmul(
                                ps[bank][:, o0:o0 + 130], src_[:, qs * 128:(qs + 1) * 128], VA[b][:, kt, :], start=(kt_i == 0 and qs % 2 == 0), stop=(kt_i == 1),
                                skip_group_check=True),
                                reads=[srck, vk], writes=[PK[bank]])
                        if kt_i == 1:
                            tb = (n // 2) % 2
                            tmk = "tmpm%d" % tb
                            for hf2 in range(2):
                                bank = pvb + hf2
                                q0 = qi * 4 + 2 * hf2
                                p.op("dve", lambda g, bank=bank, hf2=hf2, q0=q0, jb=jb, sel=sel, tb=tb: g.tensor_tensor(
                                    out=tmpm[tb][:, 2 * hf2:2 * hf2 + 2, :], in0=ps[bank][:, 0:260].rearrange("p (a b) -> p a b", b=130),
                                    in1=sel[:, q0:q0 + 2, jb:jb + 1].to_broadcast([128, 2, 130]), op=ALU.mult),
                                    reads=[PK[bank], selk], writes=[tmk])
                            p.op("pool", lambda g, tb=tb: g.tensor_tensor(out=acc[:], in0=acc[:], in1=tmpm[tb][:], op=ALU.add),
                                 reads=[tmk, "acc"], writes=["acc"])
                        if s["last"]:
                            p.op("dve", lambda g: g.reciprocal(out=rec[:], in_=acc[:, :, 128]), reads=["acc"], writes=["rec"])
                            a = (h * 2 + qi) % 2
                            p.op("dve", lambda g, a=a: g.tensor_tensor(out=ao[a][:], in0=acc[:, :, 0:128], in1=rec[:].unsqueeze(2).to_broadcast([128, 4, 128]), op=ALU.mult),
                                 reads=["acc", "rec"], writes=["ao%d" % a])
                            p.dma("sp", att_s[qi * 512:(qi + 1) * 512, h * 128:(h + 1) * 128].rearrange("(n p) d -> p n d", p=128), ao[a][:],
                                  reads=["ao%d" % a])
                            if qi == 1 and h + 2 < 8:
                                load_head(h + 2)

                    load_head(0)
                    load_head(1)
                    NS = len(steps)
                    for n in range(min(LA, NS)):
                        emit_score(n)
                    for n in range(NS):
                        if n + LA < NS:
                            emit_score(n + LA)
                        emit_pv(n)
                    p.barrier()
                    p.flush()

            if "C" in phases:
                with ExitStack() as sc:
                    gbias = T(sc, "gbias", [128, 32]); convw = T(sc, "convw", [128, 8, 3])
                    xt = T(sc, "xtC", [128, D])
                    xnT = T(sc, "xnTC", [128, 16, 512], F32R)
                    xhT = T(sc, "xhTC", [128, 16, 128], F32R)
                    attnT = T(sc, "attnTC", [128, 8, 512], F32R)
                    zprev = T(sc, "zprevC", [128, 8, 2])
                    p.dma("sp", gbias[:], gbias_d, writes=["gbias"])
                    p.dma("sp", convw[:], convw_d, writes=["convw"])
                    for hf in range(2):
                        with ExitStack() as s0:
                            g1bc = T(s0, "g1bcC%d" % hf, [128, D]); xsc = T(s0, "xscC%d" % hf, [128, D]); small = T(s0, "smallC%d" % hf, [128, 4])
                            p.dma("sp", g1bc[:], g1_d, writes=["g1bc"])
                            if hf == 0:
                                norm_tile(xhalo_d, g1bc, "g1bc", xt, xsc, small, xhT, "xhT", 0, (0, 1), "C")
                            for tt in range(4):
                                ti = hf * 4 + tt
                                norm_tile(xrot_d[ti * 128:(ti + 1) * 128, :], g1bc, "g1bc", xt, xsc, small, xnT, "xnT", tt * 128, (0, 1), "C")
                                p.dma("sp", xt[:, 0:1024], att_s[ti * 128:(ti + 1) * 128, :], writes=["xtC"])
                                for g4 in range(2):
                                    bnk = g4 % 2
                                    for i in range(4):
                                        hc = g4 * 4 + i
                                        p.op("pe", lambda g, bnk=bnk, i=i, hc=hc: g.transpose(ps[bnk][:, i * 128:(i + 1) * 128], xt[:, hc * 128:(hc + 1) * 128], ident[:]),
                                             reads=["xtC", "ident"], writes=[PK[bnk]])
                                    p.op("act", lambda g, bnk=bnk, g4=g4, tt=tt: g.copy(out=attnT[:, g4 * 4:(g4 + 1) * 4, tt * 128:(tt + 1) * 128],
                                                                                     in_=ps[bnk][:].rearrange("p (a b) -> p a b", b=128)),
                                         reads=[PK[bnk]], writes=["attnT"])
                            p.barrier()
                            p.flush()
                        with ExitStack() as sm:
                            mT = T(sm, "mTC%d" % hf, [128, 16, 512], F32R)
                            with ExitStack() as scv:
                                convT = T(scv, "convTC%d" % hf, [128, 8, 512], F32R)
                                with ExitStack() as s1:
                                    wcv = [T(s1, "wcv%d_%d" % (i, hf), [128, 16, 3, 128], F32R) for i in range(2)]
                                    z = T(s1, "zC%d" % hf, [128, 514]); csb = T(s1, "csbC%d" % hf, [128, 512]); tcv = T(s1, "tcvC%d" % hf, [128, 512])
                                    chalo = T(s1, "chaloC%d" % hf, [128, 2])
                                    def load_cv(ci_):
                                        b_ = ci_ % 2
                                        p.dma("pool", wcv[b_][:], w_cv_l[ci_], writes=["wcv%d" % b_])

                                    load_cv(0)
                                    for ci in range(8):
                                        b = ci % 2
                                        if ci + 1 < 8:
                                            load_cv(ci + 1)
                                        wk = "wcv%d" % b
                                        for s_, pb in ((0, 2), (1, 3), (2, 4)):
                                            for dc in range(16):
                                                p.op("pe", lambda g, pb=pb, b=b, dc=dc, s_=s_: g.matmul(ps[pb][:], wcv[b][:, dc, s_, :], xnT[:, dc, :], start=(dc == 0), stop=(dc == 15)),
                                                     reads=[wk, "xnT"], writes=[PK[pb]])
                                        if hf == 0:
                                            for s_, o0 in ((1, 0), (2, 2)):
                                                for dc in range(16):
                                                    p.op("pe", lambda g, b=b, dc=dc, s_=s_, o0=o0: g.matmul(ps[5][:, o0:o0 + 2], wcv[b][:, dc, s_, :], xhT[:, dc, 126:128],
                                                                                                            start=(dc == 0), stop=(dc == 15)),
                                                         reads=[wk, "xhT"], writes=[PK[5]])
                                            p.op("act", lambda g: g.copy(out=chalo[:], in_=ps[5][:, 0:2]), reads=[PK[5]], writes=["chalo"])
                                            p.op("dve", lambda g: g.tensor_tensor(out=z[:, 0:2], in0=chalo[:], in1=ps[5][:, 2:4], op=ALU.mult),
                                                 reads=["chalo", PK[5]], writes=["z"])
                                        else:
                                            p.op("dve", lambda g, ci=ci: g.tensor_copy(out=z[:, 0:2], in_=zprev[:, ci, :]), reads=["zprev"], writes=["z"])
                                        p.op("act", lambda g: g.copy(out=csb[:], in_=ps[3][:]), reads=[PK[3]], writes=["csb"])
                                        p.op("dve", lambda g: g.tensor_tensor(out=z[:, 2:514], in0=csb[:], in1=ps[4][:], op=ALU.mult), reads=["csb", PK[4]], writes=["z"])
                                        if hf == 0:
                                            p.op("pool", lambda g, ci=ci: g.tensor_copy(out=zprev[:, ci, :], in_=z[:, 512:514]), reads=["z"], writes=["zprev"])
                                        p.op("dve", lambda g, ci=ci: g.tensor_scalar(out=tcv[:], in0=z[:, 0:512], scalar1=convw[:, ci, 0:1], scalar2=None, op0=ALU.mult),
                                             reads=["z", "convw"], writes=["tcv"])
                                        p.op("dve", lambda g, ci=ci: g.scalar_tensor_tensor(out=tcv[:], in0=z[:, 1:513], scalar=convw[:, ci, 1:2], in1=tcv[:], op0=ALU.mult, op1=ALU.add),
                                             reads=["z", "convw", "tcv"], writes=["tcv"])
                                        p.op("dve", lambda g, ci=ci: g.scalar_tensor_tensor(out=tcv[:], in0=z[:, 2:514], scalar=convw[:, ci, 2:3], in1=tcv[:], op0=ALU.mult, op1=ALU.add),
                                             reads=["z", "convw", "tcv"], writes=["tcv"])
                                        p.op("dve", lambda g, ci=ci: g.tensor_tensor(out=convT[:, ci, :], in0=ps[2][:], in1=tcv[:], op=ALU.mult),
                                             reads=[PK[2], "tcv"], writes=["convT"])
                                    p.barrier()
                                    p.flush()
                                with ExitStack() as s2:
                                    wbr = [T(s2, "wbr%d_%d" % (i, hf), [128, 2, 8, 128], F32R) for i in range(2)]
                                    wgt = [T(s2, "wgt%d_%d" % (i, hf), [128, 16, 2, 128], F32R) for i in range(2)]
                                    gs = [T(s2, "gsC%d_%d" % (i, hf), [128, 512]) for i in range(2)]
                                    mm_ = [T(s2, "mmC%d_%d" % (i, hf), [128, 512]) for i in range(2)]

                                    def load_m(fc):
                                        b = fc % 2
                                        p.dma("pool", wbr[b][:], w_br_l[fc], writes=["wbr%d" % b])
                                        p.dma("pool", wgt[b][:], w_gt_l[fc], writes=["wgt%d" % b])

                                    load_m(0)
                                    for fc in range(16):
                                        b = fc % 2
                                        if fc + 1 < 16:
                                            load_m(fc + 1)
                                        bk, gk = "wbr%d" % b, "wgt%d" % b
                                        for cc in range(8):
                                            p.op("pe", lambda g, b=b, cc=cc: g.matmul(ps[2][:], wbr[b][:, 0, cc, :], convT[:, cc, :], start=(cc == 0), stop=(cc == 7)),
                                                 reads=[bk, "convT"], writes=[PK[2]])
                                        for cc in range(8):
                                            p.op("pe", lambda g, b=b, cc=cc: g.matmul(ps[3][:], wbr[b][:, 1, cc, :], attnT[:, cc, :], start=(cc == 0), stop=(cc == 7)),
                                                 reads=[bk, "attnT"], writes=[PK[3]])
                                        for gi, pb in ((0, 4), (1, 5)):
                                            for dc in range(16):
                                                p.op("pe", lambda g, b=b, dc=dc, gi=gi, pb=pb: g.matmul(ps[pb][:], wgt[b][:, dc, gi, :], xnT[:, dc, :], start=(dc == 0), stop=(dc == 15)),
                                                     reads=[gk, "xnT"], writes=[PK[pb]])
                                        p.op("act", lambda g, fc=fc: g.activation(out=gs[0][:], in_=ps[4][:], func=AF.Sigmoid, bias=gbias[:, fc:fc + 1], scale=1.0),
                                             reads=[PK[4], "gbias"], writes=["gs0"])
                                        p.op("act", lambda g, fc=fc: g.activation(out=gs[1][:], in_=ps[5][:], func=AF.Sigmoid, bias=gbias[:, 16 + fc:17 + fc], scale=1.0),
                                             reads=[PK[5], "gbias"], writes=["gs1"])
                                        p.op("dve", lambda g: g.tensor_tensor(out=mm_[0][:], in0=gs[0][:], in1=ps[2][:], op=ALU.mult), reads=["gs0", PK[2]], writes=["mm0"])
                                        p.op("dve", lambda g: g.tensor_tensor(out=mm_[1][:], in0=gs[1][:], in1=ps[3][:], op=ALU.mult), reads=["gs1", PK[3]], writes=["mm1"])
                                        p.op("pool", lambda g, fc=fc: g.tensor_tensor(out=mT[:, fc, :], in0=mm_[0][:], in1=mm_[1][:], op=ALU.add),
                                             reads=["mm0", "mm1"], writes=["mT"])
                                    p.barrier()
                                    p.flush()
                            with ExitStack() as s3:
                                wo = [T(s3, "wo%d_%d" % (i, hf), [128, 16, 256], F32R) for i in range(2)]
                                nwo = [0]

                                def load_wo(fb):
                                    b = nwo[0] % 2
                                    nwo[0] += 1
                                    p.dma("pool", wo[b][:], w_out_l[fb], writes=["wo%d" % b])
                                    return b

                                xt4 = T(s3, "xt4_%d" % hf, [128, 4, D])
                                for tt in range(4):
                                    ti = hf * 4 + tt
                                    p.dma("sp", xt4[:, tt, :], xrot_d[ti * 128:(ti + 1) * 128, :], writes=["xt4_%d" % tt])
                                bcur = load_wo(0)
                                for fb in range(8):
                                    b = bcur
                                    if fb + 1 < 8:
                                        bcur = load_wo(fb + 1)
                                    for tt in range(4):
                                        pb = 6 + (fb * 4 + tt) % 2
                                        for dc in range(16):
                                            p.op("pe", lambda g, b=b, dc=dc, pb=pb, tt=tt: g.matmul(ps[pb][:, 0:256], mT[:, dc, tt * 128:(tt + 1) * 128], wo[b][:, dc, :],
                                                                                                   start=(dc == 0), stop=(dc == 15)),
                                                 reads=["wo%d" % b, "mT"], writes=[PK[pb]])
                                        p.op("dve", lambda g, pb=pb, fb=fb, tt=tt: g.tensor_tensor(out=xt4[:, tt, fb * 256:(fb + 1) * 256], in0=ps[pb][:, 0:256],
                                                                                                  in1=xt4[:, tt, fb * 256:(fb + 1) * 256], op=ALU.add),
                                             reads=[PK[pb], "xt4_%d" % tt], writes=["xt4_%d" % tt])
                                for tt in range(4):
                                    ti = hf * 4 + tt
                                    p.dma("sp", x2_s[ti * 128:(ti + 1) * 128, :], xt4[:, tt, :], reads=["xt4_%d" % tt])
                                p.barrier()
                                p.flush()

            if "D" in phases:
                with ExitStack() as sd:
                    xbf = T(sd, "xn2Tbf", [128, 16, NT], BF16)
                    with ExitStack() as s0:
                        p.mute = "0" not in phases
                        g2bc = T(s0, "g2bc", [128, D]); x2t = T(s0, "x2t", [128, D]); junk0 = T(s0, "junk0", [128, D])
                        small = T(s0, "smallD0", [128, 4])
                        xr = T(s0, "xn2Tr", [128, 16, NT], F32R)
                        wpq = [T(s0, "wpq%d" % i, [128, 16, 128], F32R) for i in range(2)]
                        qst = [T(s0, "qst%d" % i, [128, NT]) for i in range(2)]
                        p.dma("sp", g2bc[:], g2_d, writes=["g2bc"])
                        p.dma("pool", wpq[0][:], w_pq_l[0], writes=["wpq0"])
                        p.mute = ("0" not in phases) or ("b" in phases and "a" not in phases)
                        for ti in range(8):
                            p.dma("sp", x2t[:], x2_s[ti * 128:(ti + 1) * 128, :], writes=["x2t"])
                            p.op("act", lambda g: g.activation(out=junk0[:], in_=x2t[:], func=AF.Square, accum_out=small[:, 0:1]),
                                 reads=["x2t"], writes=["junk0", "smD"])
                            p.op("act", lambda g: g.activation(out=small[:, 1:2], in_=small[:, 0:1], func=AF.Sqrt, scale=1.0 / D, bias=EPS), reads=["smD"], writes=["smD"])
                            p.op("dve", lambda g: g.reciprocal(out=small[:, 2:3], in_=small[:, 1:2]), reads=["smD"], writes=["smD"])
                            p.op("dve", lambda g: g.scalar_tensor_tensor(out=junk0[:], in0=x2t[:], scalar=small[:, 2:3], in1=g2bc[:], op0=ALU.mult, op1=ALU.mult),
                                 reads=["x2t", "smD", "g2bc", "junk0"], writes=["junk0"])
                            for g4 in range(4):
                                bnk = g4 % 2
                                for i in range(4):
                                    dc = g4 * 4 + i
                                    p.op("pe", lambda g, bnk=bnk, i=i, dc=dc: g.transpose(ps[bnk][:, i * 128:(i + 1) * 128], junk0[:, dc * 128:(dc + 1) * 128], ident[:]),
                                         reads=["junk0", "ident"], writes=[PK[bnk]])
                                p.op("act", lambda g, bnk=bnk, g4=g4, ti=ti: g.copy(out=xr[:, g4 * 4:(g4 + 1) * 4, ti * 128:(ti + 1) * 128], in_=ps[bnk][:].rearrange("p (a b) -> p a b", b=128)),
                                     reads=[PK[bnk]], writes=["xr"])
                                if "x" not in phases:
                                    p.op("pool", lambda g, g4=g4, ti=ti: g.tensor_copy(out=xbf[:, g4 * 4:(g4 + 1) * 4, ti * 128:(ti + 1) * 128], in_=fv(xr[:, g4 * 4:(g4 + 1) * 4, ti * 128:(ti + 1) * 128])),
                                         reads=["xr"], writes=["xbf"])
                        p.mute = ("0" not in phases) or ("a" in phases and "b" not in phases)
                        for fc in range(16):
                            b = fc % 2
                            if fc + 1 < 16:
                                p.dma("pool", wpq[1 - b][:], w_pq_l[fc + 1], writes=["wpq%d" % (1 - b)])
                            for half in range(2):
                                pb = 2 + (fc * 2 + half) % 4
                                for dc in range(16):
                                    p.op("pe", lambda g, b=b, dc=dc, half=half, pb=pb: g.matmul(ps[pb][:], wpq[b][:, dc, :], xr[:, dc, half * 512:(half + 1) * 512],
                                                                                               start=(dc == 0), stop=(dc == 15)),
                                         reads=["wpq%d" % b, "xr"], writes=[PK[pb]])
                                p.op("act", lambda g, b=b, half=half, pb=pb: g.copy(out=qst[b][:, half * 512:(half + 1) * 512], in_=ps[pb][:]),
                                     reads=[PK[pb]], writes=["qst%d" % b])
                            p.dma("sp", qp_s[fc], qst[b][:], reads=["qst%d" % b])
                        p.barrier()
                        p.flush()
                    with ExitStack() as s1:
                        p.mute = "1" not in phases
                        skT = T(s1, "skT", [128, 16, 128], F32R)
                        qpT = [T(s1, "qpT%d" % i, [128, 16, 128], F32R) for i in range(2)]
                        scb = T(s1, "scb", [128, 16, 128])
                        sc2 = T(s1, "sc2", [128, 16, 128])
                        cand = T(s1, "cand", [128, 8, 256])
                        stop_ = T(s1, "stop", [128, 16, 16]); itop = T(s1, "itop", [128, 16, 16], U32); itf = T(s1, "itf", [128, 16, 16])
                        i1x = T(s1, "i1x", [128, 8, 16])
                        best = T(s1, "best", [128, 8, 16]); gt = T(s1, "gt", [128, 8, 16]); zs = T(s1, "zs", [128, 8])
                        Ef = T(s1, "Ef", [128, 128]); Ei = T(s1, "Ei", [128, 128], I32); Ej = T(s1, "Ej", [128, 128], I32)
                        I1f = T(s1, "I1f", [128, 128]); I2f = T(s1, "I2f", [128, 128])
                        trT2 = [T(s1, "trT%d" % i, [128, 3, 128]) for i in range(2)]
                        posu = T(s1, "posu", [128, 8, 16], U32); posf = T(s1, "posf", [128, 128])
                        af = T(s1, "af", [128, 128]); bf = T(s1, "bf", [128, 128])
                        Ac2 = [T(s1, "Ac%d" % i, [128, 32, 128], BF16) for i in range(2)]
                        Bc2 = [T(s1, "Bc%d" % i, [128, 32, 128], BF16) for i in range(2)]
                        Wt2 = [T(s1, "Wt%d" % i, [128, 128, 128], BF16) for i in range(2)]
                        iota_bf = T(s1, "iotabf", [128, 128], BF16)
                        trTb2 = [T(s1, "trTb%d" % i, [128, 2, 128], BF16) for i in range(2)]
                        p.op("pool", lambda g: g.tensor_copy(out=iota_bf[:], in_=iota[:]), reads=["iota"], writes=["iotabf"])
                        p.dma("pool", skT[:], skT_d, writes=["skT"])
                        qp_v = qp_s.rearrange("f p t -> p f t")

                        def load_qp(ti_):
                            p.dma("pool", qpT[ti_ % 2][:], qp_v[:, :, ti_ * 128:(ti_ + 1) * 128], writes=["qpT%d" % (ti_ % 2)])

                        load_qp(0)
                        st4 = stop_[:].rearrange("p (h q) k -> p h q k", q=2)
                        it4 = itf[:].rearrange("p (h q) k -> p h q k", q=2)
                        egrid = scb[:].rearrange("p (h q) k -> p h (q k)", q=2)
                        cand2 = sc2[:].rearrange("p (h q) k -> p h (q k)", q=2)
                        iob = iota_bf[:].unsqueeze(1).to_broadcast([128, 32, 128])

                        def routing(ti):
                            qb = qpT[ti % 2]
                            qk = "qpT%d" % (ti % 2)
                            trT = trT2[ti % 2]
                            trk = "trT%d" % (ti % 2)
                            if ti + 1 < 8:
                                load_qp(ti + 1)
                            for g4 in range(4):
                                pb = 4 + g4 % 2
                                for i in range(4):
                                    hp = g4 * 4 + i
                                    p.op("pe", lambda g, pb=pb, i=i, hp=hp, qb=qb: g.matmul(ps[pb][:, i * 128:(i + 1) * 128], qb[:, hp, :], skT[:, hp, :], start=True, stop=True),
                                         reads=[qk, "skT"], writes=[PK[pb]])
                                p.op("act", lambda g, pb=pb, g4=g4: g.copy(out=scb[:, g4 * 4:(g4 + 1) * 4, :], in_=ps[pb][:].rearrange("p (a b) -> p a b", b=128)),
                                     reads=[PK[pb]], writes=["scb"])
                            yield
                            for hp in range(16):
                                p.op("dve", lambda g, hp=hp: g.max(out=stop_[:, hp, 0:8], in_=scb[:, hp, :]), reads=["scb"], writes=["stop"])
                                p.op("dve", lambda g, hp=hp: g.max_index(out=itop[:, hp, 0:8], in_max=stop_[:, hp, 0:8], in_values=scb[:, hp, :]),
                                     reads=["scb", "stop"], writes=["itop"])
                                p.op("dve", lambda g, hp=hp: g.match_replace(out=sc2[:, hp, :], in_to_replace=stop_[:, hp, 0:8], in_values=scb[:, hp, :], imm_value=NEG),
                                     reads=["scb", "stop"], writes=["sc2"])
                                p.op("dve", lambda g, hp=hp: g.max(out=stop_[:, hp, 8:16], in_=sc2[:, hp, :]), reads=["sc2"], writes=["stop"])
                                p.op("dve", lambda g, hp=hp: g.max_index(out=itop[:, hp, 8:16], in_max=stop_[:, hp, 8:16], in_values=sc2[:, hp, :]),
                                     reads=["sc2", "stop"], writes=["itop"])
                                yield
                            p.op("dve", lambda g: g.tensor_copy(out=itf[:], in_=itop[:]), reads=["itop"], writes=["itf"])
                            p.op("dve", lambda g: g.tensor_tensor(out=cand[:].rearrange("p h (a b) -> p h a b", b=16),
                                                                   in0=st4[:, :, 0, :].unsqueeze(3).to_broadcast([128, 8, 16, 16]),
                                                                   in1=st4[:, :, 1, :].unsqueeze(2).to_broadcast([128, 8, 16, 16]), op=ALU.add),
                                 reads=["stop"], writes=["cand"])
                            yield
                            for h in range(8):
                                p.op("dve", lambda g, h=h: g.max(out=best[:, h, 0:8], in_=cand[:, h, :]), reads=["cand"], writes=["best"])
                                p.op("dve", lambda g, h=h: g.max_index(out=posu[:, h, 0:8], in_max=best[:, h, 0:8], in_values=cand[:, h, :]),
                                     reads=["cand", "best"], writes=["posu"])
                                p.op("dve", lambda g, h=h: g.match_replace(out=cand2[:, h, :], in_to_replace=best[:, h, 0:8], in_values=cand[:, h, :], imm_value=NEG),
                                     reads=["cand", "best", "stop", "itop"], writes=["sc2"])
                                p.op("dve", lambda g, h=h: g.max(out=best[:, h, 8:16], in_=cand2[:, h, :]), reads=["sc2"], writes=["best"])
                                p.op("dve", lambda g, h=h: g.max_index(out=posu[:, h, 8:16], in_max=best[:, h, 8:16], in_values=cand2[:, h, :]),
                                     reads=["sc2", "best"], writes=["posu"])
                                if h % 2 == 1:
                                    yield
                            p.op("dve", lambda g: g.tensor_tensor(out=gt[:], in0=best[:], in1=best[:, :, 0:1].to_broadcast([128, 8, 16]), op=ALU.subtract),
                                 reads=["best"], writes=["gt"])
                            p.op("act", lambda g: g.activation(out=gt[:], in_=gt[:], func=AF.Exp), reads=["gt"], writes=["gt"])
                            p.op("dve", lambda g: g.tensor_reduce(out=zs[:], in_=gt[:], axis=AX.X, op=ALU.add), reads=["gt"], writes=["zs"])
                            p.op("dve", lambda g: g.reciprocal(out=zs[:], in_=zs[:]), reads=["zs"], writes=["zs"])
                            p.op("dve", lambda g: g.tensor_tensor(out=gt[:], in0=gt[:], in1=zs[:].unsqueeze(2).to_broadcast([128, 8, 16]), op=ALU.mult),
                                 reads=["gt", "zs"], writes=["gt"])
                            yield
                            p.op("dve", lambda g: g.tensor_copy(out=posf[:], in_=posu[:].rearrange("p h k -> p (h k)")), reads=["posu"], writes=["posf"])
                            p.op("dve", lambda g: g.tensor_copy(out=Ei[:], in_=posf[:]), reads=["posf"], writes=["Ei"])
                            p.op("dve", lambda g: g.tensor_single_scalar(out=Ej[:], in_=Ei[:], scalar=4, op=ALU.logical_shift_right), reads=["Ei"], writes=["Ej"])
                            p.op("dve", lambda g: g.tensor_copy(out=af[:], in_=Ej[:]), reads=["Ej"], writes=["af"])
                            p.op("dve", lambda g: g.tensor_single_scalar(out=Ej[:], in_=Ei[:], scalar=15, op=ALU.bitwise_and), reads=["Ei", "af"], writes=["Ej"])
                            p.op("dve", lambda g: g.tensor_copy(out=bf[:], in_=Ej[:]), reads=["Ej"], writes=["bf"])
                            yield
                            sc3 = scb[:].rearrange("p a b -> p (a b)").rearrange("p (s a) -> p s a", a=16)
                            sc4 = scb[:].rearrange("p a b -> p (a b)").rearrange("p (h k a) -> p h k a", k=16, a=16)
                            io16 = iota[:, 0:16].unsqueeze(1).to_broadcast([128, 128, 16])
                            for q_, (sf, sfk, dst, dk) in enumerate(((af, "af", I1f, "I1f"), (bf, "bf", I2f, "I2f"))):
                                p.op("dve", lambda g, sf=sf: g.tensor_tensor(out=sc3, in0=sf[:].unsqueeze(2).to_broadcast([128, 128, 16]), in1=io16, op=ALU.is_equal),
                                     reads=[sfk, "iota", "stop", "itop"], writes=["scb"])
                                p.op("dve", lambda g, q_=q_: g.tensor_tensor(out=sc4, in0=sc4, in1=it4[:, :, q_, :].unsqueeze(2).to_broadcast([128, 8, 16, 16]), op=ALU.mult),
                                     reads=["scb", "itf"], writes=["scb"])
                                p.op("dve", lambda g, dst=dst: g.tensor_reduce(out=dst[:], in_=sc3, axis=AX.X, op=ALU.add), reads=["scb"], writes=[dk])
                                yield
                            for i, (src_, sk) in enumerate(((I1f[:], "I1f"), (I2f[:], "I2f"), (gt[:].rearrange("p h k -> p (h k)"), "gt"))):
                                p.op("pe", lambda g, i=i, src_=src_: g.transpose(ps[6][:, i * 128:(i + 1) * 128], src_, ident[:]), reads=[sk, "ident"], writes=[PK[6]])
                            p.op("act", lambda g, trT=trT: g.copy(out=trT[:], in_=ps[6][:, 0:384].rearrange("p (a b) -> p a b", b=128)), reads=[PK[6]], writes=[trk])
                            p.op("act", lambda g, ti=ti: g.copy(out=trTb2[ti % 2][:], in_=ps[6][:, 0:256].rearrange("p (a b) -> p a b", b=128)), reads=[PK[6]], writes=["trTb%d" % (ti % 2)])
                            yield

                        ohc = [0]

                        def onehot(ti):
                            trT = trT2[ti % 2]
                            trk = "trT%d" % (ti % 2)
                            Wt = Wt2[ti % 2]
                            wtk = "Wt%d" % (ti % 2)
                            for tc in range(4):
                                t0 = tc * 32
                                ab = ohc[0] % 2
                                ohc[0] += 1
                                Ac, Bc = Ac2[ab], Bc2[ab]
                                ak, bk = "Ac%d" % ab, "Bc%d" % ab
                                p.op("dve", lambda g, t0=t0, Ac=Ac, ti=ti: g.tensor_tensor(out=Ac[:], in0=iob, in1=trTb2[ti % 2][:, 0, t0:t0 + 32].unsqueeze(2).to_broadcast([128, 32, 128]), op=ALU.is_equal),
                                     reads=["iotabf", "trTb%d" % (ti % 2)], writes=[ak])
                                p.op("pool", lambda g, t0=t0, Ac=Ac, trT=trT: g.tensor_tensor(out=Ac[:], in0=Ac[:], in1=trT[:, 2, t0:t0 + 32].unsqueeze(2).to_broadcast([128, 32, 128]), op=ALU.mult),
                                     reads=[ak, trk], writes=[ak])
                                yield
                                p.op("dve", lambda g, t0=t0, Bc=Bc, ti=ti: g.tensor_tensor(out=Bc[:], in0=iob, in1=trTb2[ti % 2][:, 1, t0:t0 + 32].unsqueeze(2).to_broadcast([128, 32, 128]), op=ALU.is_equal),
                                     reads=["iotabf", "trTb%d" % (ti % 2)], writes=[bk])
                                yield
                                for t4 in range(8):
                                    pb = 2 + t4 % 2
                                    for i in range(4):
                                        tl = t4 * 4 + i
                                        p.op("pe", lambda g, tl=tl, pb=pb, i=i, Ac=Ac, Bc=Bc: g.matmul(ps[pb][:, i * 128:(i + 1) * 128], Ac[:, tl, :], Bc[:, tl, :], start=True, stop=True),
                                             reads=[ak, bk], writes=[PK[pb]])
                                    tg = t0 + t4 * 4
                                    p.op("act", lambda g, pb=pb, tg=tg, Wt=Wt: g.copy(out=Wt[:, :, tg:tg + 4].rearrange("p i t -> p t i"), in_=ps[pb][:].rearrange("p (a b) -> p a b", b=128)),
                                         reads=[PK[pb]], writes=[wtk])
                                    if t4 % 2 == 1:
                                        yield
                            for rq in range(4):
                                p.dma("sp", Wd[rq * 32:(rq + 1) * 32, :, ti * 128:(ti + 1) * 128].rearrange("r p t -> p r t"), Wt[:, rq * 32:(rq + 1) * 32, :], reads=[wtk])
                            yield

                        def run_all(gen):
                            for _ in gen:
                                pass

                        def interleave(ga_, gb_, ra=1, rb=1):
                            alive_a, alive_b = ga_ is not None, gb_ is not None
                            while alive_a or alive_b:
                                if alive_a:
                                    for _ in range(ra):
                                        try:
                                            next(ga_)
                                        except StopIteration:
                                            alive_a = False
                                            break
                                if alive_b:
                                    for _ in range(rb):
                                        try:
                                            next(gb_)
                                        except StopIteration:
                                            alive_b = False
                                            break

                        run_all(routing(0))
                        for ti in range(8):
                            interleave(onehot(ti), routing(ti + 1) if ti + 1 < 8 else None)
                        p.barrier()
                        p.flush()
                    with ExitStack() as s2:
                        p.mute = "2" not in phases
                        acc = T(s2, "accD", [128, 8, D])
                        s2a = ExitStack()
                        Ub = [T(s2a, "Ub%d" % i, [128, 16, 128], BF16) for i in range(4)]
                        Vs = [T(s2a, "Vs%d" % i, [128, D], BF16) for i in range(8)]
                        Wg = [T(s2a, "Wg%d" % i, [128, 4, NT], BF16) for i in range(2)]
                        ga = [T(s2a, "ga%d" % i, [128, NT]) for i in range(2)]
                        act = [T(s2a, "actD%d" % i, [128, 4, NT], BF16) for i in range(2)]
                        for ti in range(8):
                            p.dma("sp", acc[:, ti, :], x2_s[ti * 128:(ti + 1) * 128, :], writes=["acc%d" % ti])

                        def load_u(r_):
                            p.dma("pool", Ub[r_ % 4][:], u_d[r_], writes=["Ub%d" % (r_ % 4)])

                        def load_v(r_):
                            p.dma("pool", Vs[r_ % 8][:], v_d[r_], writes=["Vs%d" % (r_ % 8)])

                        def load_w(g_):
                            p.dma("sp", Wg[g_ % 2][:], Wd[g_ * 4:(g_ + 1) * 4].rearrange("r p t -> p r t"), writes=["Wg%d" % (g_ % 2)])

                        def u_part(g_):
                            b = g_ % 2
                            if g_ + 1 < 32:
                                load_w(g_ + 1)
                            for rr in range(4):
                                r = g_ * 4 + rr
                                if r + 3 < 128:
                                    load_u(r + 3)
                                load_v(r)
                                a2 = r % 2
                                banks = (0, 1) if a2 == 0 else (2, 3)
                                for half in range(2):
                                    pb = banks[half]
                                    for dc in range(16):
                                        p.op("pe", lambda g, r=r, dc=dc, half=half, pb=pb: g.matmul(ps[pb][:], Ub[r % 4][:, dc, :], xbf[:, dc, half * 512:(half + 1) * 512],
                                                                                                   start=(dc == 0), stop=(dc == 15)),
                                             reads=["Ub%d" % (r % 4), "xbf"], writes=[PK[pb]])
                                for half in range(2):
                                    pb = banks[half]
                                    p.op("act", lambda g, a2=a2, half=half, pb=pb: g.activation(out=ga[a2][:, half * 512:(half + 1) * 512], in_=ps[pb][:], func=AF.Gelu),
                                         reads=[PK[pb]], writes=["ga%d" % a2])
                                p.op("dve", lambda g, a2=a2, rr=rr, b=b: g.tensor_tensor(out=act[b][:, rr, :], in0=ga[a2][:], in1=Wg[b][:, rr, :], op=ALU.mult),
                                     reads=["ga%d" % a2, "Wg%d" % b], writes=["act%d" % b])

                        def v_part(g_):
                            b = g_ % 2
                            for ti in range(8):
                                for half in range(2):
                                    pbs = (4, 5) if half == 0 else (6, 7)
                                    for rr in range(4):
                                        sl = (g_ * 4 + rr) % 8
                                        for j in range(2):
                                            c0 = (half * 2 + j) * 512
                                            p.op("pe", lambda g, rr=rr, j=j, ti=ti, b=b, c0=c0, pbs=pbs, sl=sl: g.matmul(ps[pbs[j]][:], act[b][:, rr, ti * 128:(ti + 1) * 128], Vs[sl][:, c0:c0 + 512],
                                                                                                                        start=(rr == 0), stop=(rr == 3)),
                                                 reads=["act%d" % b, "Vs%d" % sl], writes=[PK[pbs[j]]])
                                    for j in range(2):
                                        c0 = (half * 2 + j) * 512
                                        p.op("dve", lambda g, j=j, ti=ti, c0=c0, pbs=pbs: g.tensor_tensor(out=acc[:, ti, c0:c0 + 512], in0=ps[pbs[j]][:], in1=acc[:, ti, c0:c0 + 512], op=ALU.add),
                                             reads=[PK[pbs[j]], "acc%d" % ti], writes=["acc%d" % ti])

                        load_w(0)
                        for r_ in range(3):
                            load_u(r_)
                        u_part(0)
                        for g_ in range(32):
                            if g_ + 1 < 32:
                                u_part(g_ + 1)
                            v_part(g_)
                        p.barrier()
                        p.flush()
                        s2a.close()
                        g3bc = T(s2, "g3bc", [128, D]); yo = T(s2, "yo", [128, D]); small = T(s2, "smallD2", [128, 4])
                        p.dma("sp", g3bc[:], g3_d, writes=["g3bc"])
                        for ti in range(8):
                            p.op("act", lambda g, ti=ti: g.activation(out=yo[:], in_=acc[:, ti, :], func=AF.Square, accum_out=small[:, 0:1]), reads=["acc%d" % ti], writes=["yo", "smD"])
                            p.op("act", lambda g: g.activation(out=small[:, 1:2], in_=small[:, 0:1], func=AF.Sqrt, scale=1.0 / D, bias=EPS), reads=["smD"], writes=["smD"])
                            p.op("dve", lambda g: g.reciprocal(out=small[:, 2:3], in_=small[:, 1:2]), reads=["smD"], writes=["smD"])
                            p.op("dve", lambda g, ti=ti: g.scalar_tensor_tensor(out=yo[:], in0=acc[:, ti, :], scalar=small[:, 2:3], in1=g3bc[:], op0=ALU.mult, op1=ALU.mult),
                                 reads=["acc%d" % ti, "smD", "g3bc", "yo"], writes=["yo"])
                            p.dma("sp", out_d[ti * 128:(ti + 1) * 128, :], yo[:], reads=["yo"])
                        p.barrier()
                        p.flush()
            p.barrier()
            p.flush()
    return nc


def _consts():
    ident = np.eye(128, dtype=np.float32)
    rmat = np.zeros((128, 128), np.float32)
    for i in range(16):
        rmat[i + 16, i] = -1.0
        rmat[i, i + 16] = 1.0
    inv_freq = 1.0 / (500000.0 ** (np.arange(0, 32, 2, dtype=np.float32) / 32.0))
    invf2 = np.zeros((128, 1), np.float32)
    invf2[:32, 0] = np.tile(inv_freq, 2) / (2.0 * np.pi)
    iota128 = np.broadcast_to(np.arange(128, dtype=np.float32)[None, :], (128, 128)).copy()
    return ident, rmat, invf2, iota128


def _masks(j):
    past = np.full((128, 8, 16), NEG, np.float32)
    own = np.zeros((128, 8, 16), np.float32)
    for ti in range(8):
        qb = 4 * j + ti // 2
        for rb in range(16):
            tb = (4 * j + rb) % 16
            if tb < qb:
                past[:, ti, rb] = 0.0
            if tb == qb:
                own[:, ti, rb] = 1.0
    return past, own


def make_in_maps(x, positions, attn_norm_g, w_in, gate_bias, conv_w, w_branch_conv, w_branch_attn,
                 w_out, ffn_norm_g, w_peer_query, peer_sub_keys, peer_u, peer_v, final_norm_g):
    f = lambda a: np.ascontiguousarray(np.asarray(a), dtype=np.float32)
    x = f(x)
    positions = np.asarray(positions).astype(np.int32)
    ident, rmat, invf2, iota128 = _consts()
    bc = lambda v: np.ascontiguousarray(np.broadcast_to(f(v).reshape(1, D), (128, D)))
    shared = dict(
        ident=ident, rmat=rmat, invf2=invf2, iota128=iota128,
        g1bc=bc(attn_norm_g[0]), g2bc=bc(ffn_norm_g[0]), g3bc=bc(final_norm_g),
        gbias=np.ascontiguousarray(f(gate_bias[0]).reshape(32, 128).T),
        convw=np.ascontiguousarray(f(conv_w[0]).reshape(3, 8, 128).transpose(2, 1, 0)),
        skT=np.ascontiguousarray(f(peer_sub_keys[0]).reshape(16, 128, 128).transpose(2, 0, 1)),
        w_qkv_l=np.ascontiguousarray(f(w_in[0])[:, 3072:6144].reshape(16, 128, 6, 512).transpose(2, 1, 0, 3)),
        w_cv_l=np.ascontiguousarray(f(w_in[0])[:, 0:3072].reshape(16, 128, 3, 8, 128).transpose(3, 1, 0, 2, 4)),
        w_gt_l=np.ascontiguousarray(f(w_in[0])[:, 6144:10240].reshape(16, 128, 2, 16, 128).transpose(3, 1, 0, 2, 4)),
        w_br_l=np.ascontiguousarray(np.stack([f(w_branch_conv[0]), f(w_branch_attn[0])]).reshape(2, 8, 128, 16, 128).transpose(3, 2, 0, 1, 4)),
        w_out_l=np.ascontiguousarray(f(w_out[0]).reshape(16, 128, 8, 256).transpose(2, 1, 0, 3)),
        w_pq_l=np.ascontiguousarray(f(w_peer_query[0]).reshape(16, 128, 16, 128).transpose(2, 1, 0, 3)),
        u_l=np.ascontiguousarray(f(peer_u[0]).reshape(128, 128, 16, 128).transpose(1, 3, 2, 0)),
        v_l=np.ascontiguousarray(f(peer_v[0]).reshape(128, 128, D).transpose(1, 0, 2)),
    )
    in_maps = []
    for c in range(8):
        b, j = c // 4, c % 4
        m = dict(shared)
        m["xrot"] = np.ascontiguousarray(np.roll(x[b], -j * 1024, axis=0))
        if j == 0:
            m["xhalo"] = np.zeros((128, D), np.float32)
        else:
            m["xhalo"] = np.ascontiguousarray(x[b, j * 1024 - 128:j * 1024])
        prot = np.roll(positions[b], -j * 1024)
        m["posb"] = np.ascontiguousarray(np.broadcast_to(prot[None, :], (128, SEQ))).astype(np.int32)
        past, own = _masks(j)
        m["pastmask"] = past
        m["ownmask"] = own
        in_maps.append(m)
    return in_maps


def kernel(**inputs):
    in_maps = make_in_maps(**inputs)
    nc = build_nc()
    res = run_bass_kernel_spmd(nc, in_maps, core_ids=list(range(8)))
    out = np.zeros((2, SEQ, D), np.float32)
    for c in range(8):
        b, j = c // 4, c % 4
        out[b, j * 1024:(j + 1) * 1024] = res.results[c]["out"]
    return out
```

```python
import os
from contextlib import ExitStack

import numpy as np
import concourse.bass as bass
import concourse.mybir as mybir
from concourse.bass_utils import run_bass_kernel_spmd

F32 = mybir.dt.float32
F32R = mybir.dt.float32r
BF16 = mybir.dt.bfloat16
I32 = mybir.dt.int32
U32 = mybir.dt.uint32
ALU = mybir.AluOpType
AF = mybir.ActivationFunctionType
AX = mybir.AxisListType

ENGS = ("pe", "act", "dve", "pool", "sp")
BLK = {"pe": "tensor", "act": "scalar", "dve": "vector", "pool": "gpsimd", "sp": "sync"}

D = 2048
NT = 1024
SEQ = 4096
INW = 10240
EPS = 1e-6
MAGIC = 12582912.0
NEG = -1.0e30


class Prog:
    def __init__(self, nc, es, n_dma_sems=48):
        self.nc = nc
        self.ops = {e: [] for e in ENGS}
        self.esem = {e: es.enter_context(nc.semaphore("s_" + e)) for e in ENGS}
        self.ecnt = {e: 0 for e in ENGS}
        self.dsem = [es.enter_context(nc.semaphore("d%d" % i)) for i in range(n_dma_sems)]
        self.dcnt = [0] * n_dma_sems
        self.dnext = 0
        self.waited = {e: {} for e in ENGS}
        self.lastw = {}
        self.readers = {}
        self.block = None
        self.mute = False

    def _sem(self, k):
        return self.esem[k] if isinstance(k, str) else self.dsem[k]

    def _need(self, e, ticket, waits):
        if ticket is None:
            return
        k, v = ticket
        if k == e and e == "pe":
            return
        if self.waited[e].get(k, 0) >= v:
            return
        if waits.get(k, 0) < v:
            waits[k] = v

    def op(self, e, fn, reads=(), writes=(), dma=False):
        if self.mute:
            return None
        waits = {}
        for r in reads:
            self._need(e, self.lastw.get(r), waits)
        for w in writes:
            self._need(e, self.lastw.get(w), waits)
            for t in self.readers.get(w, ()):
                self._need(e, t, waits)
        if dma:
            i = self.dnext
            self.dnext = (self.dnext + 1) % len(self.dsem)
            if self.dcnt[i] > 0:
                self._need(e, (i, self.dcnt[i]), waits)
            self.dcnt[i] += 16
            ticket = (i, self.dcnt[i])
        else:
            self.ecnt[e] += 1
            ticket = (e, self.ecnt[e])
        for k, v in waits.items():
            self.waited[e][k] = v
        self.ops[e].append((list(waits.items()), fn, ticket, dma))
        for r in reads:
            self.readers.setdefault(r, []).append(ticket)
        for w in writes:
            self.lastw[w] = ticket
            self.readers[w] = []
        return ticket

    def dma(self, e, out, in_, reads=(), writes=()):
        kw = {"max_dma_last_dim": 4096} if e == "pool" else {}
        return self.op(e, lambda g: g.dma_start(out=out, in_=in_, **kw), reads, writes, dma=True)

    def barrier(self):
        for e in ENGS:
            waits = {}
            for k in ENGS:
                if self.ecnt[k] > 0:
                    self._need(e, (k, self.ecnt[k]), waits)
            for i, c in enumerate(self.dcnt):
                if c > 0:
                    self._need(e, (i, c), waits)
            for k, v in waits.items():
                self.waited[e][k] = v
            if waits:
                self.ops[e].append((list(waits.items()), None, None, False))
        self.lastw = {}
        self.readers = {}

    def flush(self):
        for e in ENGS:
            lst = self.ops[e]
            if not lst:
                continue
            self.ops[e] = []

            def body(eng, lst=lst):
                for waits, fn, ticket, dma in lst:
                    for k, v in waits:
                        eng.wait_ge(self._sem(k), v)
                    if fn is None:
                        continue
                    ins = fn(eng)
                    k, v = ticket
                    ins.then_inc(self._sem(k), 16 if dma else 1)

            getattr(self.block, BLK[e])(body)


def fv(ap):
    return ap.bitcast(F32)


def build_nc(dbg=False, phases="ABCD012"):
    nc = bass.Bass("TRN2", target_bir_lowering=False)

    def din(name, shape, dt=F32):
        return nc.dram_tensor(name, list(shape), dt, kind="ExternalInput").ap()

    skind = "ExternalOutput" if dbg else "Internal"

    def dscr(name, shape):
        return nc.dram_tensor(name, list(shape), F32, kind=skind).ap()

    xrot_d = din("xrot", [SEQ, D])
    xhalo_d = din("xhalo", [128, D])
    posb_d = din("posb", [128, SEQ], I32)
    pastm_d = din("pastmask", [128, 8, 16])
    ownm_d = din("ownmask", [128, 8, 16])
    ident_d = din("ident", [128, 128])
    rmat_d = din("rmat", [128, 128])
    invf_d = din("invf2", [128, 1])
    iota_d = din("iota128", [128, 128])
    g1_d = din("g1bc", [128, D])
    g2_d = din("g2bc", [128, D])
    g3_d = din("g3bc", [128, D])
    gbias_d = din("gbias", [128, 32])
    convw_d = din("convw", [128, 8, 3])
    skT_d = din("skT", [128, 16, 128])
    w_qkv_l = din("w_qkv_l", [6, 128, 16, 512])
    w_cv_l = din("w_cv_l", [8, 128, 16, 3, 128])
    w_gt_l = din("w_gt_l", [16, 128, 16, 2, 128])
    w_br_l = din("w_br_l", [16, 128, 2, 8, 128])
    w_out_l = din("w_out_l", [8, 128, 16, 256])
    w_pq_l = din("w_pq_l", [16, 128, 16, 128])
    u_d = din("u_l", [128, 128, 16, 128])
    v_d = din("v_l", [128, 128, D])
    out_d = nc.dram_tensor("out", [NT, D], F32, kind="ExternalOutput").ap()

    kT_s = dscr("kT_s", [8, 128, SEQ])
    qT_s = dscr("qT_s", [8, 128, NT])
    v_s = dscr("v_s", [SEQ, 1024])
    att_s = dscr("att_s", [NT, 1024])
    x2_s = dscr("x2_s", [NT, D])
    qp_s = nc.dram_tensor("qp_s", [16, 128, NT], F32, kind="Internal").ap()
    Wd = nc.dram_tensor("Wd_s", [128, 128, NT], BF16, kind="Internal").ap()

    QSCALE = 128.0 ** -0.5

    with ExitStack() as es:
        p = Prog(nc, es)

        tcount = [0]

        def T(st, name, shape, dt=F32):
            tcount[0] += 1
            return st.enter_context(nc.sbuf_tensor("%s_%d" % (name, tcount[0]), list(shape), dt))

        ps = [es.enter_context(nc.psum_tensor("psb%d" % i, [128, 512], F32)) for i in range(8)]
        PK = ["ps%d" % i for i in range(8)]
        ident = T(es, "ident", [128, 128])
        iota = T(es, "iota", [128, 128])

        with nc.Block() as block:
            p.block = block
            p.dma("sp", ident[:], ident_d, writes=["ident"])
            p.dma("sp", iota[:], iota_d, writes=["iota"])

            def norm_tile(src_rows, gbc, gkey, xt, xsc, small, dstT, dkey, col0, pst, tagn):
                p.dma("sp", xt[:], src_rows, writes=["xt" + tagn])
                p.op("act", lambda g: g.activation(out=xsc[:], in_=xt[:], func=AF.Square, accum_out=small[:, 0:1]),
                     reads=["xt" + tagn], writes=["xsc" + tagn, "sm" + tagn])
                p.op("act", lambda g: g.activation(out=small[:, 1:2], in_=small[:, 0:1], func=AF.Sqrt, scale=1.0 / D, bias=EPS),
                     reads=["sm" + tagn], writes=["sm" + tagn])
                p.op("dve", lambda g: g.reciprocal(out=small[:, 2:3], in_=small[:, 1:2]), reads=["sm" + tagn], writes=["sm" + tagn])
                p.op("dve", lambda g: g.scalar_tensor_tensor(out=xsc[:], in0=xt[:], scalar=small[:, 2:3], in1=gbc[:],
                                                              op0=ALU.mult, op1=ALU.mult),
                     reads=["xt" + tagn, "sm" + tagn, gkey], writes=["xsc" + tagn])
                for g4 in range(4):
                    b = pst[g4 % 2]
                    for i in range(4):
                        dc = g4 * 4 + i
                        p.op("pe", lambda g, b=b, i=i, dc=dc: g.transpose(ps[b][:, i * 128:(i + 1) * 128], xsc[:, dc * 128:(dc + 1) * 128], ident[:]),
                             reads=["xsc" + tagn, "ident"], writes=[PK[b]])
                    p.op("act", lambda g, b=b, g4=g4: g.copy(out=dstT[:, g4 * 4:(g4 + 1) * 4, col0:col0 + 128],
                                                             in_=ps[b][:].rearrange("p (a b) -> p a b", b=128)),
                         reads=[PK[b]], writes=[dkey])

            if "A" in phases:
                with ExitStack() as sa:
                    g1bc = T(sa, "g1bc", [128, D])
                    posb = T(sa, "posb", [128, 512], I32)
                    invf = T(sa, "invf", [128, 1])
                    rmat = T(sa, "rmat", [128, 128], F32R)
                    xt = T(sa, "xtA", [128, D]); xsc = T(sa, "xscA", [128, D]); small = T(sa, "smallA", [128, 4])
                    xnT = [T(sa, "xnTA%d" % i, [128, 16, 512], F32R) for i in range(2)]
                    wb = [T(sa, "wbA%d" % i, [128, 16, 512], F32R) for i in range(2)]
                    cs = [T(sa, "csA%d" % i, [128, 2, 512]) for i in range(2)]
                    tmp = [T(sa, "tmpA%d" % i, [128, 512]) for i in range(4)]
                    ksb = [T(sa, "ksbA%d" % i, [128, 512], F32R) for i in range(2)]
                    t1 = [T(sa, "t1A%d" % i, [128, 512]) for i in range(2)]
                    t2 = [T(sa, "t2A%d" % i, [128, 512]) for i in range(2)]
                    kst = [T(sa, "kstA%d" % i, [128, 512]) for i in range(2)]
                    vst = [T(sa, "vstA%d" % i, [128, 512]) for i in range(2)]
                    p.dma("sp", g1bc[:], g1_d, writes=["g1bc"])
                    p.dma("sp", invf[:], invf_d, writes=["invf"])
                    p.dma("pool", rmat[:], rmat_d, writes=["rmat"])

                    wcount = [0]
                    kcount = [0]
                    vcount = [0]

                    def blocks_of(ck):
                        bl = [("k", i) for i in range(2)] + [("v", i) for i in range(2)]
                        if ck < 2:
                            bl += [("q", i) for i in range(2)]
                        return bl

                    allblocks = [(ck, kind, bi) for ck in range(8) for (kind, bi) in blocks_of(ck)]

                    def load_w(idx):
                        ck, kind, bi = allblocks[idx]
                        blk = {"q": 0, "k": 2, "v": 4}[kind] + bi
                        b = idx % 2
                        p.dma("pool", wb[b][:], w_qkv_l[blk], writes=["wb%d" % b])

                    def prep(ck):
                        xb = xnT[ck % 2]
                        xk = "xnT%d" % (ck % 2)
                        tok0 = ck * 512
                        for tt in range(4):
                            ti = ck * 4 + tt
                            norm_tile(xrot_d[ti * 128:(ti + 1) * 128, :], g1bc, "g1bc", xt, xsc, small, xb, xk, tt * 128, (0, 1), "A")
                            yield
                        c = cs[ck % 2]
                        ck_ = "cs%d" % (ck % 2)
                        y, r_, f_, yc = tmp
                        p.dma("sp", posb[:], posb_d[:, tok0:tok0 + 512], writes=["posb"])
                        p.op("dve", lambda g: g.tensor_copy(out=y[:], in_=posb[:]), reads=["posb"], writes=["tmp0"])
                        p.op("dve", lambda g: g.tensor_scalar(out=y[:], in0=y[:], scalar1=invf[:, 0:1], scalar2=None, op0=ALU.mult),
                             reads=["tmp0", "invf"], writes=["tmp0"])
                        p.op("dve", lambda g: g.tensor_scalar(out=r_[:], in0=y[:], scalar1=MAGIC, scalar2=None, op0=ALU.add), reads=["tmp0"], writes=["tmp1"])
                        p.op("dve", lambda g: g.tensor_scalar(out=r_[:], in0=r_[:], scalar1=-MAGIC, scalar2=None, op0=ALU.add), reads=["tmp1"], writes=["tmp1"])
                        p.op("dve", lambda g: g.tensor_tensor(out=f_[:], in0=y[:], in1=r_[:], op=ALU.subtract), reads=["tmp0", "tmp1"], writes=["tmp2"])
                        p.op("act", lambda g, c=c: g.activation(out=c[:, 1, :], in_=f_[:], func=AF.Sin, scale=6.283185),
                             reads=["tmp2"], writes=[ck_])
                        p.op("dve", lambda g: g.tensor_scalar(out=yc[:], in0=y[:], scalar1=0.25, scalar2=None, op0=ALU.add), reads=["tmp0"], writes=["tmp3"])
                        p.op("dve", lambda g: g.tensor_scalar(out=r_[:], in0=yc[:], scalar1=MAGIC, scalar2=None, op0=ALU.add), reads=["tmp3"], writes=["tmp1"])
                        p.op("dve", lambda g: g.tensor_scalar(out=r_[:], in0=r_[:], scalar1=-MAGIC, scalar2=None, op0=ALU.add), reads=["tmp1"], writes=["tmp1"])
                        p.op("dve", lambda g: g.tensor_tensor(out=f_[:], in0=yc[:], in1=r_[:], op=ALU.subtract), reads=["tmp3", "tmp1"], writes=["tmp2"])
                        p.op("act", lambda g, c=c: g.activation(out=c[:, 0, :], in_=f_[:], func=AF.Sin, scale=6.283185),
                             reads=["tmp2"], writes=[ck_])
                        yield

                    pending = []

                    def flush_pending():
                        while pending:
                            pending.pop(0)()

                    def make_tail(a, c, ck_, kind, h, tok0):
                        def tail():
                            pb2 = 4 + a
                            p.op("pe", lambda g, a=a, pb2=pb2: g.matmul(ps[pb2][:], rmat[:], ksb[a][:], start=True, stop=True),
                                 reads=["rmat", "ksb%d" % a], writes=[PK[pb2]])
                            p.op("dve", lambda g, a=a, c=c: g.tensor_tensor(out=t1[a][:], in0=fv(ksb[a][:]), in1=c[:, 0, :], op=ALU.mult),
                                 reads=["ksb%d" % a, ck_], writes=["t1%d" % a])
                            p.op("dve", lambda g, a=a, c=c, pb2=pb2: g.tensor_tensor(out=t2[a][:], in0=ps[pb2][:], in1=c[:, 1, :], op=ALU.mult),
                                 reads=[PK[pb2], ck_], writes=["t2%d" % a])
                            p.op("pool", lambda g, a=a: g.tensor_tensor(out=kst[a][:], in0=t1[a][:], in1=t2[a][:], op=ALU.add),
                                 reads=["t1%d" % a, "t2%d" % a], writes=["kst%d" % a])
                            if kind == "k":
                                p.dma("sp", kT_s[h, :, tok0:tok0 + 512], kst[a][:], reads=["kst%d" % a])
                            else:
                                p.dma("sp", qT_s[h, :, tok0:tok0 + 512], kst[a][:], reads=["kst%d" % a])
                        return tail

                    for _ in prep(0):
                        pass
                    load_w(0)
                    nxt = None
                    for idx, (ck, kind, bi) in enumerate(allblocks):
                        xb = xnT[ck % 2]
                        xk = "xnT%d" % (ck % 2)
                        tok0 = ck * 512
                        if (kind, bi) == ("k", 0):
                            flush_pending()
                            nxt = prep(ck + 1) if ck + 1 < 8 else None
                        if idx + 1 < len(allblocks):
                            load_w(idx + 1)
                        b = idx % 2
                        wk = "wb%d" % b
                        c = cs[ck % 2]
                        ck_ = "cs%d" % (ck % 2)
                        if kind in ("k", "q"):
                            for hh in range(4):
                                h = bi * 4 + hh
                                a = kcount[0] % 2
                                kcount[0] += 1
                                pb = 2 + a
                                for dc in range(16):
                                    p.op("pe", lambda g, pb=pb, b=b, dc=dc, hh=hh, xb=xb: g.matmul(ps[pb][:], wb[b][:, dc, hh * 128:(hh + 1) * 128], xb[:, dc, :],
                                                                                                   start=(dc == 0), stop=(dc == 15)),
                                         reads=[wk, xk], writes=[PK[pb]])
                                flush_pending()
                                sc_ = QSCALE if kind == "q" else 1.0
                                p.op("act", lambda g, a=a, pb=pb, sc_=sc_: g.activation(out=ksb[a][:], in_=ps[pb][:], func=AF.Copy, scale=sc_),
                                     reads=[PK[pb]], writes=["ksb%d" % a])
                                pending.append(make_tail(a, c, ck_, kind, h, tok0))
                        else:
                            for tt in range(4):
                                a = vcount[0] % 2
                                vcount[0] += 1
                                pb = 6 + a
                                for dc in range(16):
                                    p.op("pe", lambda g, pb=pb, b=b, dc=dc, tt=tt, xb=xb: g.matmul(ps[pb][:], xb[:, dc, tt * 128:(tt + 1) * 128], wb[b][:, dc, :],
                                                                                                   start=(dc == 0), stop=(dc == 15)),
                                         reads=[wk, xk], writes=[PK[pb]])
                                if tt == 0:
                                    flush_pending()
                                p.op("act", lambda g, a=a, pb=pb: g.copy(out=vst[a][:], in_=ps[pb][:]), reads=[PK[pb]], writes=["vst%d" % a])
                                r0 = ck * 512 + tt * 128
                                p.dma("sp", v_s[r0:r0 + 128, bi * 512:(bi + 1) * 512], vst[a][:], reads=["vst%d" % a])
                        for _ in range(2):
                            if nxt is not None:
                                try:
                                    next(nxt)
                                except StopIteration:
                                    nxt = None
                    flush_pending()
                    if nxt is not None:
                        for _ in nxt:
                            pass
                    p.barrier()
                    p.flush()

            if "B" in phases:
                with ExitStack() as sb:
                    pastm = T(sb, "pastm", [128, 8, 16]); ownm = T(sb, "ownm", [128, 8, 16])
                    KT = [T(sb, "KT%d" % i, [128, SEQ], F32R) for i in range(2)]
                    VA = [T(sb, "VA%d" % i, [128, 32, 130], BF16) for i in range(2)]
                    QT = [T(sb, "QT%d" % i, [128, NT], F32R) for i in range(2)]
                    km = T(sb, "km", [128, 16]); kmr = T(sb, "kmr", [128, 16], F32R)
                    gm = T(sb, "gm", [128, 16]); m8 = T(sb, "m8", [128, 8]); thr = T(sb, "thr", [128, 1])
                    sel2 = [T(sb, "sel%d" % i, [128, 8, 16]) for i in range(2)]
                    pT = [T(sb, "pT%d" % i, [128, 512], BF16) for i in range(4)]
                    pM = [T(sb, "pM%d" % i, [128, 512], BF16) for i in range(4)]
                    acc = T(sb, "acc", [128, 4, 130]); rec = T(sb, "rec", [128, 4])
                    tmpm = [T(sb, "tmpm%d" % i, [128, 4, 130]) for i in range(2)]
                    ao = [T(sb, "ao%d" % i, [128, 4, 128]) for i in range(2)]
                    ones2 = T(sb, "ones2", [128, 32, 2])
                    p.dma("sp", pastm[:], pastm_d, writes=["pastm"])
                    p.dma("sp", ownm[:], ownm_d, writes=["ownm"])
                    p.op("pool", lambda g: g.memset(ones2[:, :, 0:1], 1.0), writes=["ones2"])
                    p.op("pool", lambda g: g.memset(ones2[:, :, 1:2], 0.0), writes=["ones2"])
                    for i in range(2):
                        p.op("pool", lambda g, i=i: g.tensor_copy(out=VA[i][:, :, 128:130], in_=ones2[:]), reads=["ones2"], writes=["VA%d" % i])

                    v_hv = v_s.rearrange("(n p) (h d) -> h p n d", p=128, d=128)

                    def load_head(h):
                        b = h % 2
                        p.dma("pool", KT[b][:], kT_s[h], writes=["KT%d" % b])
                        p.dma("pool", VA[b][:, :, 0:128], v_hv[h], writes=["VA%d" % b])
                        p.dma("pool", QT[b][:], qT_s[h], writes=["QT%d" % b])

                    def head_prologue(h):
                        b = h % 2
                        sel = sel2[b]
                        selk = "sel%d" % b
                        kk, qk = "KT%d" % b, "QT%d" % b
                        p.op("dve", lambda g, b=b: g.tensor_reduce(out=km[:], in_=fv(KT[b][:]).rearrange("p (n k) -> p n k", k=256), axis=AX.X, op=ALU.add),
                             reads=[kk], writes=["km"])
                        p.op("act", lambda g: g.activation(out=kmr[:], in_=km[:], func=AF.Copy, scale=1.0 / 256.0), reads=["km"], writes=["kmr"])
                        for ti in range(8):
                            p.op("pe", lambda g, b=b, ti=ti: g.matmul(ps[0][:, 0:16], QT[b][:, ti * 128:(ti + 1) * 128], kmr[:], start=True, stop=True),
                                 reads=[qk, "kmr"], writes=[PK[0]])
                            p.op("dve", lambda g, ti=ti: g.tensor_tensor(out=gm[:], in0=ps[0][:, 0:16], in1=pastm[:, ti, :], op=ALU.add),
                                 reads=[PK[0], "pastm"], writes=["gm"])
                            p.op("dve", lambda g: g.max(out=m8[:], in_=gm[:]), reads=["gm"], writes=["m8"])
                            p.op("dve", lambda g: g.tensor_scalar(out=thr[:], in0=m8[:, 2:3], scalar1=-1.0e29, scalar2=None, op0=ALU.max),
                                 reads=["m8"], writes=["thr"])
                            p.op("dve", lambda g, ti=ti, sel=sel: g.tensor_scalar(out=sel[:, ti, :], in0=gm[:], scalar1=thr[:, 0:1], scalar2=None, op0=ALU.is_ge),
                                 reads=["gm", "thr"], writes=[selk])
                            p.op("dve", lambda g, ti=ti, sel=sel: g.tensor_tensor(out=sel[:, ti, :], in0=sel[:, ti, :], in1=ownm[:, ti, :], op=ALU.max),
                                 reads=[selk, "ownm"], writes=[selk])

                    steps = []
                    for h in range(8):
                        for qi in range(2):
                            jbs = [jb for jb in range(16) if not (jb < 4 and jb > 2 * qi + 1)]
                            for jb in jbs:
                                for kt_i in range(2):
                                    steps.append(dict(h=h, qi=qi, jb=jb, kt_i=kt_i, kt=2 * jb + kt_i,
                                                      first=(jb == jbs[0] and kt_i == 0), last=(jb == jbs[-1] and kt_i == 1)))
                    LA = 2
                    srcs = {}

                    def emit_score(n):
                        s = steps[n]
                        h, qi, jb, kt = s["h"], s["qi"], s["jb"], s["kt"]
                        b = h % 2
                        if s["first"] and qi == 0:
                            head_prologue(h)
                        kk, qk = "KT%d" % b, "QT%d" % b
                        sb_ = 1 + (n % 3)
                        pt = pT[n % 4]
                        ptk = "pT%d" % (n % 4)
                        p.op("pe", lambda g, b=b, kt=kt, qi=qi, sb_=sb_: g.matmul(ps[sb_][:], KT[b][:, kt * 128:(kt + 1) * 128], QT[b][:, qi * 512:(qi + 1) * 512],
                                                                                start=True, stop=True),
                             reads=[kk, qk], writes=[PK[sb_]])
                        p.op("act", lambda g, pt=pt, sb_=sb_: g.activation(out=pt[:], in_=ps[sb_][:], func=AF.Exp), reads=[PK[sb_]], writes=[ptk])
                        src_, srck = pt, ptk
                        if jb < 4:
                            base = qi * 512 - kt * 128
                            if base - 127 < 0:
                                pm = pM[n % 4]
                                pmk = "pM%d" % (n % 4)
                                p.op("pool", lambda g, pm=pm, pt=pt, base=base: g.affine_select(out=pm[:], in_=pt[:], pattern=[[1, 512]], compare_op=ALU.is_ge,
                                                                                              fill=0.0, base=base, channel_multiplier=-1),
                                     reads=[ptk], writes=[pmk])
                                src_, srck = pm, pmk
                        srcs[n] = (src_, srck)

                    def emit_pv(n):
                        s = steps[n]
                        h, qi, jb, kt, kt_i = s["h"], s["qi"], s["jb"], s["kt"], s["kt_i"]
                        b = h % 2
                        sel = sel2[b]
                        selk = "sel%d" % b
                        vk = "VA%d" % b
                        src_, srck = srcs.pop(n)
                        if s["first"]:
                            p.op("pool", lambda g: g.memset(acc[:], 0.0), writes=["acc"])
                        pvb = 4 + ((n // 2) % 2) * 2
                        for qs in range(4):
                            bank = pvb + qs // 2
                            o0 = (qs % 2) * 130
                            p.op("pe", lambda g, src_=src_, qs=qs, bank=bank, o0=o0, kt=kt, kt_i=kt_i, b=b: g.matmul(
                                ps[bank][:, o0:o0 + 130], src_[:, qs * 128:(qs + 1) * 128], VA[b][:, kt, :], start=(kt_i == 0 and qs % 2 == 0), stop=(kt_i == 1),
                                skip_group_check=True),
                                reads=[srck, vk], writes=[PK[bank]])
                        if kt_i == 1:
                            tb = (n // 2) % 2
                            tmk = "tmpm%d" % tb
                            for hf2 in range(2):
                                bank = pvb + hf2
                                q0 = qi * 4 + 2 * hf2
                                p.op("dve", lambda g, bank=bank, hf2=hf2, q0=q0, jb=jb, sel=sel, tb=tb: g.tensor_tensor(
                                    out=tmpm[tb][:, 2 * hf2:2 * hf2 + 2, :], in0=ps[bank][:, 0:260].rearrange("p (a b) -> p a b", b=130),
                                    in1=sel[:, q0:q0 + 2, jb:jb + 1].to_broadcast([128, 2, 130]), op=ALU.mult),
                                    reads=[PK[bank], selk], writes=[tmk])
                            p.op("pool", lambda g, tb=tb: g.tensor_tensor(out=acc[:], in0=acc[:], in1=tmpm[tb][:], op=ALU.add),
                                 reads=[tmk, "acc"], writes=["acc"])
                        if s["last"]:
                            p.op("dve", lambda g: g.reciprocal(out=rec[:], in_=acc[:, :, 128]), reads=["acc"], writes=["rec"])
                            a = (h * 2 + qi) % 2
                            p.op("dve", lambda g, a=a: g.tensor_tensor(out=ao[a][:], in0=acc[:, :, 0:128], in1=rec[:].unsqueeze(2).to_broadcast([128, 4, 128]), op=ALU.mult),
                                 reads=["acc", "rec"], writes=["ao%d" % a])
                            p.dma("sp", att_s[qi * 512:(qi + 1) * 512, h * 128:(h + 1) * 128].rearrange("(n p) d -> p n d", p=128), ao[a][:],
                                  reads=["ao%d" % a])
                            if qi == 1 and h + 2 < 8:
                                load_head(h + 2)

                    load_head(0)
                    load_head(1)
                    NS = len(steps)
                    for n in range(min(LA, NS)):
                        emit_score(n)
                    for n in range(NS):
                        if n + LA < NS:
                            emit_score(n + LA)
                        emit_pv(n)
                    p.barrier()
                    p.flush()

            if "C" in phases:
                with ExitStack() as sc:
                    gbias = T(sc, "gbias", [128, 32]); convw = T(sc, "convw", [128, 8, 3])
                    xt = T(sc, "xtC", [128, D])
                    xnT = T(sc, "xnTC", [128, 16, 512], F32R)
                    xhT = T(sc, "xhTC", [128, 16, 128], F32R)
                    attnT = T(sc, "attnTC", [128, 8, 512], F32R)
                    zprev = T(sc, "zprevC", [128, 8, 2])
                    p.dma("sp", gbias[:], gbias_d, writes=["gbias"])
                    p.dma("sp", convw[:], convw_d, writes=["convw"])
                    for hf in range(2):
                        with ExitStack() as s0:
                            g1bc = T(s0, "g1bcC%d" % hf, [128, D]); xsc = T(s0, "xscC%d" % hf, [128, D]); small = T(s0, "smallC%d" % hf, [128, 4])
                            p.dma("sp", g1bc[:], g1_d, writes=["g1bc"])
                            if hf == 0:
                                norm_tile(xhalo_d, g1bc, "g1bc", xt, xsc, small, xhT, "xhT", 0, (0, 1), "C")
                            for tt in range(4):
                                ti = hf * 4 + tt
                                norm_tile(xrot_d[ti * 128:(ti + 1) * 128, :], g1bc, "g1bc", xt, xsc, small, xnT, "xnT", tt * 128, (0, 1), "C")
                                p.dma("sp", xt[:, 0:1024], att_s[ti * 128:(ti + 1) * 128, :], writes=["xtC"])
                                for g4 in range(2):
                                    bnk = g4 % 2
                                    for i in range(4):
                                        hc = g4 * 4 + i
                                        p.op("pe", lambda g, bnk=bnk, i=i, hc=hc: g.transpose(ps[bnk][:, i * 128:(i + 1) * 128], xt[:, hc * 128:(hc + 1) * 128], ident[:]),
                                             reads=["xtC", "ident"], writes=[PK[bnk]])
                                    p.op("act", lambda g, bnk=bnk, g4=g4, tt=tt: g.copy(out=attnT[:, g4 * 4:(g4 + 1) * 4, tt * 128:(tt + 1) * 128],
                                                                                     in_=ps[bnk][:].rearrange("p (a b) -> p a b", b=128)),
                                         reads=[PK[bnk]], writes=["attnT"])
                            p.barrier()
                            p.flush()
                        with ExitStack() as sm:
                            mT = T(sm, "mTC%d" % hf, [128, 16, 512], F32R)
                            with ExitStack() as scv:
                                convT = T(scv, "convTC%d" % hf, [128, 8, 512], F32R)
                                with ExitStack() as s1:
                                    wcv = [T(s1, "wcv%d_%d" % (i, hf), [128, 16, 3, 128], F32R) for i in range(2)]
                                    z = T(s1, "zC%d" % hf, [128, 514]); csb = T(s1, "csbC%d" % hf, [128, 512]); tcv = T(s1, "tcvC%d" % hf, [128, 512])
                                    chalo = T(s1, "chaloC%d" % hf, [128, 2])
                                    def load_cv(ci_):
                                        b_ = ci_ % 2
                                        p.dma("pool", wcv[b_][:], w_cv_l[ci_], writes=["wcv%d" % b_])

                                    load_cv(0)
                                    for ci in range(8):
                                        b = ci % 2
                                        if ci + 1 < 8:
                                            load_cv(ci + 1)
                                        wk = "wcv%d" % b
                                        for s_, pb in ((0, 2), (1, 3), (2, 4)):
                                            for dc in range(16):
                                                p.op("pe", lambda g, pb=pb, b=b, dc=dc, s_=s_: g.matmul(ps[pb][:], wcv[b][:, dc, s_, :], xnT[:, dc, :], start=(dc == 0), stop=(dc == 15)),
                                                     reads=[wk, "xnT"], writes=[PK[pb]])
                                        if hf == 0:
                                            for s_, o0 in ((1, 0), (2, 2)):
                                                for dc in range(16):
                                                    p.op("pe", lambda g, b=b, dc=dc, s_=s_, o0=o0: g.matmul(ps[5][:, o0:o0 + 2], wcv[b][:, dc, s_, :], xhT[:, dc, 126:128],
                                                                                                            start=(dc == 0), stop=(dc == 15)),
                                                         reads=[wk, "xhT"], writes=[PK[5]])
                                            p.op("act", lambda g: g.copy(out=chalo[:], in_=ps[5][:, 0:2]), reads=[PK[5]], writes=["chalo"])
                                            p.op("dve", lambda g: g.tensor_tensor(out=z[:, 0:2], in0=chalo[:], in1=ps[5][:, 2:4], op=ALU.mult),
                                                 reads=["chalo", PK[5]], writes=["z"])
                                        else:
                                            p.op("dve", lambda g, ci=ci: g.tensor_copy(out=z[:, 0:2], in_=zprev[:, ci, :]), reads=["zprev"], writes=["z"])
                                        p.op("act", lambda g: g.copy(out=csb[:], in_=ps[3][:]), reads=[PK[3]], writes=["csb"])
                                        p.op("dve", lambda g: g.tensor_tensor(out=z[:, 2:514], in0=csb[:], in1=ps[4][:], op=ALU.mult), reads=["csb", PK[4]], writes=["z"])
                                        if hf == 0:
                                            p.op("pool", lambda g, ci=ci: g.tensor_copy(out=zprev[:, ci, :], in_=z[:, 512:514]), reads=["z"], writes=["zprev"])
                                        p.op("dve", lambda g, ci=ci: g.tensor_scalar(out=tcv[:], in0=z[:, 0:512], scalar1=convw[:, ci, 0:1], scalar2=None, op0=ALU.mult),
                                             reads=["z", "convw"], writes=["tcv"])
                                        p.op("dve", lambda g, ci=ci: g.scalar_tensor_tensor(out=tcv[:], in0=z[:, 1:513], scalar=convw[:, ci, 1:2], in1=tcv[:], op0=ALU.mult, op1=ALU.add),
                                             reads=["z", "convw", "tcv"], writes=["tcv"])
                                        p.op("dve", lambda g, ci=ci: g.scalar_tensor_tensor(out=tcv[:], in0=z[:, 2:514], scalar=convw[:, ci, 2:3], in1=tcv[:], op0=ALU.mult, op1=ALU.add),
                                             reads=["z", "convw", "tcv"], writes=["tcv"])
                                        p.op("dve", lambda g, ci=ci: g.tensor_tensor(out=convT[:, ci, :], in0=ps[2][:], in1=tcv[:], op=ALU.mult),
                                             reads=[PK[2], "tcv"], writes=["convT"])
                                    p.barrier()
                                    p.flush()
                                with ExitStack() as s2:
                                    wbr = [T(s2, "wbr%d_%d" % (i, hf), [128, 2, 8, 128], F32R) for i in range(2)]
                                    wgt = [T(s2, "wgt%d_%d" % (i, hf), [128, 16, 2, 128], F32R) for i in range(2)]
                                    gs = [T(s2, "gsC%d_%d" % (i, hf), [128, 512]) for i in range(2)]
                                    mm_ = [T(s2, "mmC%d_%d" % (i, hf), [128, 512]) for i in range(2)]

                                    def load_m(fc):
                                        b = fc % 2
                                        p.dma("pool", wbr[b][:], w_br_l[fc], writes=["wbr%d" % b])
                                        p.dma("pool", wgt[b][:], w_gt_l[fc], writes=["wgt%d" % b])

                                    load_m(0)
                                    for fc in range(16):
                                        b = fc % 2
                                        if fc + 1 < 16:
                                            load_m(fc + 1)
                                        bk, gk = "wbr%d" % b, "wgt%d" % b
                                        for cc in range(8):
                                            p.op("pe", lambda g, b=b, cc=cc: g.matmul(ps[2][:], wbr[b][:, 0, cc, :], convT[:, cc, :], start=(cc == 0), stop=(cc == 7)),
                                                 reads=[bk, "convT"], writes=[PK[2]])
                                        for cc in range(8):
                                            p.op("pe", lambda g, b=b, cc=cc: g.matmul(ps[3][:], wbr[b][:, 1, cc, :], attnT[:, cc, :], start=(cc == 0), stop=(cc == 7)),
                                                 reads=[bk, "attnT"], writes=[PK[3]])
                                        for gi, pb in ((0, 4), (1, 5)):
                                            for dc in range(16):
                                                p.op("pe", lambda g, b=b, dc=dc, gi=gi, pb=pb: g.matmul(ps[pb][:], wgt[b][:, dc, gi, :], xnT[:, dc, :], start=(dc == 0), stop=(dc == 15)),
                                                     reads=[gk, "xnT"], writes=[PK[pb]])
                                        p.op("act", lambda g, fc=fc: g.activation(out=gs[0][:], in_=ps[4][:], func=AF.Sigmoid, bias=gbias[:, fc:fc + 1], scale=1.0),
                                             reads=[PK[4], "gbias"], writes=["gs0"])
                                        p.op("act", lambda g, fc=fc: g.activation(out=gs[1][:], in_=ps[5][:], func=AF.Sigmoid, bias=gbias[:, 16 + fc:17 + fc], scale=1.0),
                                             reads=[PK[5], "gbias"], writes=["gs1"])
                                        p.op("dve", lambda g: g.tensor_tensor(out=mm_[0][:], in0=gs[0][:], in1=ps[2][:], op=ALU.mult), reads=["gs0", PK[2]], writes=["mm0"])
                                        p.op("dve", lambda g: g.tensor_tensor(out=mm_[1][:], in0=gs[1][:], in1=ps[3][:], op=ALU.mult), reads=["gs1", PK[3]], writes=["mm1"])
                                        p.op("pool", lambda g, fc=fc: g.tensor_tensor(out=mT[:, fc, :], in0=mm_[0][:], in1=mm_[1][:], op=ALU.add),
                                             reads=["mm0", "mm1"], writes=["mT"])
                                    p.barrier()
                                    p.flush()
                            with ExitStack() as s3:
                                wo = [T(s3, "wo%d_%d" % (i, hf), [128, 16, 256], F32R) for i in range(2)]
                                nwo = [0]

                                def load_wo(fb):
                                    b = nwo[0] % 2
                                    nwo[0] += 1
                                    p.dma("pool", wo[b][:], w_out_l[fb], writes=["wo%d" % b])
                                    return b

                                xt4 = T(s3, "xt4_%d" % hf, [128, 4, D])
                                for tt in range(4):
                                    ti = hf * 4 + tt
                                    p.dma("sp", xt4[:, tt, :], xrot_d[ti * 128:(ti + 1) * 128, :], writes=["xt4_%d" % tt])
                                bcur = load_wo(0)
                                for fb in range(8):
                                    b = bcur
                                    if fb + 1 < 8:
                                        bcur = load_wo(fb + 1)
                                    for tt in range(4):
                                        pb = 6 + (fb * 4 + tt) % 2
                                        for dc in range(16):
                                            p.op("pe", lambda g, b=b, dc=dc, pb=pb, tt=tt: g.matmul(ps[pb][:, 0:256], mT[:, dc, tt * 128:(tt + 1) * 128], wo[b][:, dc, :],
                                                                                                   start=(dc == 0), stop=(dc == 15)),
                                                 reads=["wo%d" % b, "mT"], writes=[PK[pb]])
                                        p.op("dve", lambda g, pb=pb, fb=fb, tt=tt: g.tensor_tensor(out=xt4[:, tt, fb * 256:(fb + 1) * 256], in0=ps[pb][:, 0:256],
                                                                                                  in1=xt4[:, tt, fb * 256:(fb + 1) * 256], op=ALU.add),
                                             reads=[PK[pb], "xt4_%d" % tt], writes=["xt4_%d" % tt])
                                for tt in range(4):
                                    ti = hf * 4 + tt
                                    p.dma("sp", x2_s[ti * 128:(ti + 1) * 128, :], xt4[:, tt, :], reads=["xt4_%d" % tt])
                                p.barrier()
                                p.flush()

            if "D" in phases:
                with ExitStack() as sd:
                    xbf = T(sd, "xn2Tbf", [128, 16, NT], BF16)
                    with ExitStack() as s0:
                        p.mute = "0" not in phases
                        g2bc = T(s0, "g2bc", [128, D]); x2t = T(s0, "x2t", [128, D]); junk0 = T(s0, "junk0", [128, D])
                        small = T(s0, "smallD0", [128, 4])
                        xr = T(s0, "xn2Tr", [128, 16, NT], F32R)
                        wpq = [T(s0, "wpq%d" % i, [128, 16, 128], F32R) for i in range(2)]
                        qst = [T(s0, "qst%d" % i, [128, NT]) for i in range(2)]
                        p.dma("sp", g2bc[:], g2_d, writes=["g2bc"])
                        p.dma("pool", wpq[0][:], w_pq_l[0], writes=["wpq0"])
                        p.mute = ("0" not in phases) or ("b" in phases and "a" not in phases)
                        for ti in range(8):
                            p.dma("sp", x2t[:], x2_s[ti * 128:(ti + 1) * 128, :], writes=["x2t"])
                            p.op("act", lambda g: g.activation(out=junk0[:], in_=x2t[:], func=AF.Square, accum_out=small[:, 0:1]),
                                 reads=["x2t"], writes=["junk0", "smD"])
                            p.op("act", lambda g: g.activation(out=small[:, 1:2], in_=small[:, 0:1], func=AF.Sqrt, scale=1.0 / D, bias=EPS), reads=["smD"], writes=["smD"])
                            p.op("dve", lambda g: g.reciprocal(out=small[:, 2:3], in_=small[:, 1:2]), reads=["smD"], writes=["smD"])
                            p.op("dve", lambda g: g.scalar_tensor_tensor(out=junk0[:], in0=x2t[:], scalar=small[:, 2:3], in1=g2bc[:], op0=ALU.mult, op1=ALU.mult),
                                 reads=["x2t", "smD", "g2bc", "junk0"], writes=["junk0"])
                            for g4 in range(4):
                                bnk = g4 % 2
                                for i in range(4):
                                    dc = g4 * 4 + i
                                    p.op("pe", lambda g, bnk=bnk, i=i, dc=dc: g.transpose(ps[bnk][:, i * 128:(i + 1) * 128], junk0[:, dc * 128:(dc + 1) * 128], ident[:]),
                                         reads=["junk0", "ident"], writes=[PK[bnk]])
                                p.op("act", lambda g, bnk=bnk, g4=g4, ti=ti: g.copy(out=xr[:, g4 * 4:(g4 + 1) * 4, ti * 128:(ti + 1) * 128], in_=ps[bnk][:].rearrange("p (a b) -> p a b", b=128)),
                                     reads=[PK[bnk]], writes=["xr"])
                                if "x" not in phases:
                                    p.op("pool", lambda g, g4=g4, ti=ti: g.tensor_copy(out=xbf[:, g4 * 4:(g4 + 1) * 4, ti * 128:(ti + 1) * 128], in_=fv(xr[:, g4 * 4:(g4 + 1) * 4, ti * 128:(ti + 1) * 128])),
                                         reads=["xr"], writes=["xbf"])
                        p.mute = ("0" not in phases) or ("a" in phases and "b" not in phases)
                        for fc in range(16):
                            b = fc % 2
                            if fc + 1 < 16:
                                p.dma("pool", wpq[1 - b][:], w_pq_l[fc + 1], writes=["wpq%d" % (1 - b)])
                            for half in range(2):
                                pb = 2 + (fc * 2 + half) % 4
                                for dc in range(16):
                                    p.op("pe", lambda g, b=b, dc=dc, half=half, pb=pb: g.matmul(ps[pb][:], wpq[b][:, dc, :], xr[:, dc, half * 512:(half + 1) * 512],
                                                                                               start=(dc == 0), stop=(dc == 15)),
                                         reads=["wpq%d" % b, "xr"], writes=[PK[pb]])
                                p.op("act", lambda g, b=b, half=half, pb=pb: g.copy(out=qst[b][:, half * 512:(half + 1) * 512], in_=ps[pb][:]),
                                     reads=[PK[pb]], writes=["qst%d" % b])
                            p.dma("sp", qp_s[fc], qst[b][:], reads=["qst%d" % b])
                        p.barrier()
                        p.flush()
                    with ExitStack() as s1:
                        p.mute = "1" not in phases
                        skT = T(s1, "skT", [128, 16, 128], F32R)
                        qpT = [T(s1, "qpT%d" % i, [128, 16, 128], F32R) for i in range(2)]
                        scb = T(s1, "scb", [128, 16, 128])
                        sc2 = T(s1, "sc2", [128, 16, 128])
                        cand = T(s1, "cand", [128, 8, 256])
                        stop_ = T(s1, "stop", [128, 16, 16]); itop = T(s1, "itop", [128, 16, 16], U32); itf = T(s1, "itf", [128, 16, 16])
                        i1x = T(s1, "i1x", [128, 8, 16])
                        best = T(s1, "best", [128, 8, 16]); gt = T(s1, "gt", [128, 8, 16]); zs = T(s1, "zs", [128, 8])
                        Ef = T(s1, "Ef", [128, 128]); Ei = T(s1, "Ei", [128, 128], I32); Ej = T(s1, "Ej", [128, 128], I32)
                        I1f = T(s1, "I1f", [128, 128]); I2f = T(s1, "I2f", [128, 128])
                        trT2 = [T(s1, "trT%d" % i, [128, 3, 128]) for i in range(2)]
                        posu = T(s1, "posu", [128, 8, 16], U32); posf = T(s1, "posf", [128, 128])
                        af = T(s1, "af", [128, 128]); bf = T(s1, "bf", [128, 128])
                        Ac2 = [T(s1, "Ac%d" % i, [128, 32, 128], BF16) for i in range(2)]
                        Bc2 = [T(s1, "Bc%d" % i, [128, 32, 128], BF16) for i in range(2)]
                        Wt2 = [T(s1, "Wt%d" % i, [128, 128, 128], BF16) for i in range(2)]
                        iota_bf = T(s1, "iotabf", [128, 128], BF16)
                        trTb2 = [T(s1, "trTb%d" % i, [128, 2, 128], BF16) for i in range(2)]
                        p.op("pool", lambda g: g.tensor_copy(out=iota_bf[:], in_=iota[:]), reads=["iota"], writes=["iotabf"])
                        p.dma("pool", skT[:], skT_d, writes=["skT"])
                        qp_v = qp_s.rearrange("f p t -> p f t")

                        def load_qp(ti_):
                            p.dma("pool", qpT[ti_ % 2][:], qp_v[:, :, ti_ * 128:(ti_ + 1) * 128], writes=["qpT%d" % (ti_ % 2)])

                        load_qp(0)
                        st4 = stop_[:].rearrange("p (h q) k -> p h q k", q=2)
                        it4 = itf[:].rearrange("p (h q) k -> p h q k", q=2)
                        egrid = scb[:].rearrange("p (h q) k -> p h (q k)", q=2)
                        cand2 = sc2[:].rearrange("p (h q) k -> p h (q k)", q=2)
                        iob = iota_bf[:].unsqueeze(1).to_broadcast([128, 32, 128])

                        def routing(ti):
                            qb = qpT[ti % 2]
                            qk = "qpT%d" % (ti % 2)
                            trT = trT2[ti % 2]
                            trk = "trT%d" % (ti % 2)
                            if ti + 1 < 8:
                                load_qp(ti + 1)
                            for g4 in range(4):
                                pb = 4 + g4 % 2
                                for i in range(4):
                                    hp = g4 * 4 + i
                                    p.op("pe", lambda g, pb=pb, i=i, hp=hp, qb=qb: g.matmul(ps[pb][:, i * 128:(i + 1) * 128], qb[:, hp, :], skT[:, hp, :], start=True, stop=True),
                                         reads=[qk, "skT"], writes=[PK[pb]])
                                p.op("act", lambda g, pb=pb, g4=g4: g.copy(out=scb[:, g4 * 4:(g4 + 1) * 4, :], in_=ps[pb][:].rearrange("p (a b) -> p a b", b=128)),
                                     reads=[PK[pb]], writes=["scb"])
                            yield
                            for hp in range(16):
                                p.op("dve", lambda g, hp=hp: g.max(out=stop_[:, hp, 0:8], in_=scb[:, hp, :]), reads=["scb"], writes=["stop"])
                                p.op("dve", lambda g, hp=hp: g.max_index(out=itop[:, hp, 0:8], in_max=stop_[:, hp, 0:8], in_values=scb[:, hp, :]),
                                     reads=["scb", "stop"], writes=["itop"])
                                p.op("dve", lambda g, hp=hp: g.match_replace(out=sc2[:, hp, :], in_to_replace=stop_[:, hp, 0:8], in_values=scb[:, hp, :], imm_value=NEG),
                                     reads=["scb", "stop"], writes=["sc2"])
                                p.op("dve", lambda g, hp=hp: g.max(out=stop_[:, hp, 8:16], in_=sc2[:, hp, :]), reads=["sc2"], writes=["stop"])
                                p.op("dve", lambda g, hp=hp: g.max_index(out=itop[:, hp, 8:16], in_max=stop_[:, hp, 8:16], in_values=sc2[:, hp, :]),
                                     reads=["sc2", "stop"], writes=["itop"])
                                yield
                            p.op("dve", lambda g: g.tensor_copy(out=itf[:], in_=itop[:]), reads=["itop"], writes=["itf"])
                            p.op("dve", lambda g: g.tensor_tensor(out=cand[:].rearrange("p h (a b) -> p h a b", b=16),
                                                                   in0=st4[:, :, 0, :].unsqueeze(3).to_broadcast([128, 8, 16, 16]),
                                                                   in1=st4[:, :, 1, :].unsqueeze(2).to_broadcast([128, 8, 16, 16]), op=ALU.add),
                                 reads=["stop"], writes=["cand"])
                            yield
                            for h in range(8):
                                p.op("dve", lambda g, h=h: g.max(out=best[:, h, 0:8], in_=cand[:, h, :]), reads=["cand"], writes=["best"])
                                p.op("dve", lambda g, h=h: g.max_index(out=posu[:, h, 0:8], in_max=best[:, h, 0:8], in_values=cand[:, h, :]),
                                     reads=["cand", "best"], writes=["posu"])
                                p.op("dve", lambda g, h=h: g.match_replace(out=cand2[:, h, :], in_to_replace=best[:, h, 0:8], in_values=cand[:, h, :], imm_value=NEG),
                                     reads=["cand", "best", "stop", "itop"], writes=["sc2"])
                                p.op("dve", lambda g, h=h: g.max(out=best[:, h, 8:16], in_=cand2[:, h, :]), reads=["sc2"], writes=["best"])
                                p.op("dve", lambda g, h=h: g.max_index(out=posu[:, h, 8:16], in_max=best[:, h, 8:16], in_values=cand2[:, h, :]),
                                     reads=["sc2", "best"], writes=["posu"])
                                if h % 2 == 1:
                                    yield
                            p.op("dve", lambda g: g.tensor_tensor(out=gt[:], in0=best[:], in1=best[:, :, 0:1].to_broadcast([128, 8, 16]), op=ALU.subtract),
                                 reads=["best"], writes=["gt"])
                            p.op("act", lambda g: g.activation(out=gt[:], in_=gt[:], func=AF.Exp), reads=["gt"], writes=["gt"])
                            p.op("dve", lambda g: g.tensor_reduce(out=zs[:], in_=gt[:], axis=AX.X, op=ALU.add), reads=["gt"], writes=["zs"])
                            p.op("dve", lambda g: g.reciprocal(out=zs[:], in_=zs[:]), reads=["zs"], writes=["zs"])
                            p.op("dve", lambda g: g.tensor_tensor(out=gt[:], in0=gt[:], in1=zs[:].unsqueeze(2).to_broadcast([128, 8, 16]), op=ALU.mult),
                                 reads=["gt", "zs"], writes=["gt"])
                            yield
                            p.op("dve", lambda g: g.tensor_copy(out=posf[:], in_=posu[:].rearrange("p h k -> p (h k)")), reads=["posu"], writes=["posf"])
                            p.op("dve", lambda g: g.tensor_copy(out=Ei[:], in_=posf[:]), reads=["posf"], writes=["Ei"])
                            p.op("dve", lambda g: g.tensor_single_scalar(out=Ej[:], in_=Ei[:], scalar=4, op=ALU.logical_shift_right), reads=["Ei"], writes=["Ej"])
                            p.op("dve", lambda g: g.tensor_copy(out=af[:], in_=Ej[:]), reads=["Ej"], writes=["af"])
                            p.op("dve", lambda g: g.tensor_single_scalar(out=Ej[:], in_=Ei[:], scalar=15, op=ALU.bitwise_and), reads=["Ei", "af"], writes=["Ej"])
                            p.op("dve", lambda g: g.tensor_copy(out=bf[:], in_=Ej[:]), reads=["Ej"], writes=["bf"])
                            yield
                            sc3 = scb[:].rearrange("p a b -> p (a b)").rearrange("p (s a) -> p s a", a=16)
                            sc4 = scb[:].rearrange("p a b -> p (a b)").rearrange("p (h k a) -> p h k a", k=16, a=16)
                            io16 = iota[:, 0:16].unsqueeze(1).to_broadcast([128, 128, 16])
                            for q_, (sf, sfk, dst, dk) in enumerate(((af, "af", I1f, "I1f"), (bf, "bf", I2f, "I2f"))):
                                p.op("dve", lambda g, sf=sf: g.tensor_tensor(out=sc3, in0=sf[:].unsqueeze(2).to_broadcast([128, 128, 16]), in1=io16, op=ALU.is_equal),
                                     reads=[sfk, "iota", "stop", "itop"], writes=["scb"])
                                p.op("dve", lambda g, q_=q_: g.tensor_tensor(out=sc4, in0=sc4, in1=it4[:, :, q_, :].unsqueeze(2).to_broadcast([128, 8, 16, 16]), op=ALU.mult),
                                     reads=["scb", "itf"], writes=["scb"])
                                p.op("dve", lambda g, dst=dst: g.tensor_reduce(out=dst[:], in_=sc3, axis=AX.X, op=ALU.add), reads=["scb"], writes=[dk])
                                yield
                            for i, (src_, sk) in enumerate(((I1f[:], "I1f"), (I2f[:], "I2f"), (gt[:].rearrange("p h k -> p (h k)"), "gt"))):
                                p.op("pe", lambda g, i=i, src_=src_: g.transpose(ps[6][:, i * 128:(i + 1) * 128], src_, ident[:]), reads=[sk, "ident"], writes=[PK[6]])
                            p.op("act", lambda g, trT=trT: g.copy(out=trT[:], in_=ps[6][:, 0:384].rearrange("p (a b) -> p a b", b=128)), reads=[PK[6]], writes=[trk])
                            p.op("act", lambda g, ti=ti: g.copy(out=trTb2[ti % 2][:], in_=ps[6][:, 0:256].rearrange("p (a b) -> p a b", b=128)), reads=[PK[6]], writes=["trTb%d" % (ti % 2)])
                            yield

                        ohc = [0]

                        def onehot(ti):
                            trT = trT2[ti % 2]
                            trk = "trT%d" % (ti % 2)
                            Wt = Wt2[ti % 2]
                            wtk = "Wt%d" % (ti % 2)
                            for tc in range(4):
                                t0 = tc * 32
                                ab = ohc[0] % 2
                                ohc[0] += 1
                                Ac, Bc = Ac2[ab], Bc2[ab]
                                ak, bk = "Ac%d" % ab, "Bc%d" % ab
                                p.op("dve", lambda g, t0=t0, Ac=Ac, ti=ti: g.tensor_tensor(out=Ac[:], in0=iob, in1=trTb2[ti % 2][:, 0, t0:t0 + 32].unsqueeze(2).to_broadcast([128, 32, 128]), op=ALU.is_equal),
                                     reads=["iotabf", "trTb%d" % (ti % 2)], writes=[ak])
                                p.op("pool", lambda g, t0=t0, Ac=Ac, trT=trT: g.tensor_tensor(out=Ac[:], in0=Ac[:], in1=trT[:, 2, t0:t0 + 32].unsqueeze(2).to_broadcast([128, 32, 128]), op=ALU.mult),
                                     reads=[ak, trk], writes=[ak])
                                yield
                                p.op("dve", lambda g, t0=t0, Bc=Bc, ti=ti: g.tensor_tensor(out=Bc[:], in0=iob, in1=trTb2[ti % 2][:, 1, t0:t0 + 32].unsqueeze(2).to_broadcast([128, 32, 128]), op=ALU.is_equal),
                                     reads=["iotabf", "trTb%d" % (ti % 2)], writes=[bk])
                                yield
                                for t4 in range(8):
                                    pb = 2 + t4 % 2
                                    for i in range(4):
                                        tl = t4 * 4 + i
                                        p.op("pe", lambda g, tl=tl, pb=pb, i=i, Ac=Ac, Bc=Bc: g.matmul(ps[pb][:, i * 128:(i + 1) * 128], Ac[:, tl, :], Bc[:, tl, :], start=True, stop=True),
                                             reads=[ak, bk], writes=[PK[pb]])
                                    tg = t0 + t4 * 4
                                    p.op("act", lambda g, pb=pb, tg=tg, Wt=Wt: g.copy(out=Wt[:, :, tg:tg + 4].rearrange("p i t -> p t i"), in_=ps[pb][:].rearrange("p (a b) -> p a b", b=128)),
                                         reads=[PK[pb]], writes=[wtk])
                                    if t4 % 2 == 1:
                                        yield
                            for rq in range(4):
                                p.dma("sp", Wd[rq * 32:(rq + 1) * 32, :, ti * 128:(ti + 1) * 128].rearrange("r p t -> p r t"), Wt[:, rq * 32:(rq + 1) * 32, :], reads=[wtk])
                            yield

                        def run_all(gen):
                            for _ in gen:
                                pass

                        def interleave(ga_, gb_, ra=1, rb=1):
                            alive_a, alive_b = ga_ is not None, gb_ is not None
                            while alive_a or alive_b:
                                if alive_a:
                                    for _ in range(ra):
                                        try:
                                            next(ga_)
                                        except StopIteration:
                                            alive_a = False
                                            break
                                if alive_b:
                                    for _ in range(rb):
                                        try:
                                            next(gb_)
                                        except StopIteration:
                                            alive_b = False
                                            break

                        run_all(routing(0))
                        for ti in range(8):
                            interleave(onehot(ti), routing(ti + 1) if ti + 1 < 8 else None)
                        p.barrier()
                        p.flush()
                    with ExitStack() as s2:
                        p.mute = "2" not in phases
                        acc = T(s2, "accD", [128, 8, D])
                        s2a = ExitStack()
                        Ub = [T(s2a, "Ub%d" % i, [128, 16, 128], BF16) for i in range(6)]
                        Vs = [T(s2a, "Vs%d" % i, [128, D], BF16) for i in range(8)]
                        Wg = [T(s2a, "Wg%d" % i, [128, 4, NT], BF16) for i in range(2)]
                        ga = [T(s2a, "ga%d" % i, [128, NT]) for i in range(2)]
                        act = [T(s2a, "actD%d" % i, [128, 4, NT], BF16) for i in range(2)]
                        for ti in range(8):
                            p.dma("sp", acc[:, ti, :], x2_s[ti * 128:(ti + 1) * 128, :], writes=["acc%d" % ti])

                        def load_u(r_):
                            p.dma("pool", Ub[r_ % 6][:], u_d[r_], writes=["Ub%d" % (r_ % 6)])

                        def load_v(r_):
                            p.dma("pool", Vs[r_ % 8][:], v_d[r_], writes=["Vs%d" % (r_ % 8)])

                        def load_w(g_):
                            p.dma("sp", Wg[g_ % 2][:], Wd[g_ * 4:(g_ + 1) * 4].rearrange("r p t -> p r t"), writes=["Wg%d" % (g_ % 2)])

                        def u_part(g_):
                            b = g_ % 2
                            if g_ + 1 < 32:
                                load_w(g_ + 1)
                            for rr in range(4):
                                r = g_ * 4 + rr
                                if r + 5 < 128:
                                    load_u(r + 5)
                                load_v(r)
                                a2 = r % 2
                                banks = (0, 1) if a2 == 0 else (2, 3)
                                for half in range(2):
                                    pb = banks[half]
                                    for dc in range(16):
                                        p.op("pe", lambda g, r=r, dc=dc, half=half, pb=pb: g.matmul(ps[pb][:], Ub[r % 6][:, dc, :], xbf[:, dc, half * 512:(half + 1) * 512],
                                                                                                   start=(dc == 0), stop=(dc == 15)),
                                             reads=["Ub%d" % (r % 6), "xbf"], writes=[PK[pb]])
                                for half in range(2):
                                    pb = banks[half]
                                    p.op("act", lambda g, a2=a2, half=half, pb=pb: g.activation(out=ga[a2][:, half * 512:(half + 1) * 512], in_=ps[pb][:], func=AF.Gelu),
                                         reads=[PK[pb]], writes=["ga%d" % a2])
                                p.op("dve", lambda g, a2=a2, rr=rr, b=b: g.tensor_tensor(out=act[b][:, rr, :], in0=ga[a2][:], in1=Wg[b][:, rr, :], op=ALU.mult),
                                     reads=["ga%d" % a2, "Wg%d" % b], writes=["act%d" % b])

                        def v_part(g_):
                            b = g_ % 2
                            for ti in range(8):
                                for half in range(2):
                                    pbs = (4, 5) if half == 0 else (6, 7)
                                    for rr in range(4):
                                        sl = (g_ * 4 + rr) % 8
                                        for j in range(2):
                                            c0 = (half * 2 + j) * 512
                                            p.op("pe", lambda g, rr=rr, j=j, ti=ti, b=b, c0=c0, pbs=pbs, sl=sl: g.matmul(ps[pbs[j]][:], act[b][:, rr, ti * 128:(ti + 1) * 128], Vs[sl][:, c0:c0 + 512],
                                                                                                                        start=(rr == 0), stop=(rr == 3)),
                                                 reads=["act%d" % b, "Vs%d" % sl], writes=[PK[pbs[j]]])
                                    for j in range(2):
                                        c0 = (half * 2 + j) * 512
                                        p.op("dve", lambda g, j=j, ti=ti, c0=c0, pbs=pbs: g.tensor_tensor(out=acc[:, ti, c0:c0 + 512], in0=ps[pbs[j]][:], in1=acc[:, ti, c0:c0 + 512], op=ALU.add),
                                             reads=[PK[pbs[j]], "acc%d" % ti], writes=["acc%d" % ti])

                        load_w(0)
                        for r_ in range(5):
                            load_u(r_)
                        u_part(0)
                        for g_ in range(32):
                            if g_ + 1 < 32:
                                u_part(g_ + 1)
                            v_part(g_)
                        p.barrier()
                        p.flush()
                        s2a.close()
                        g3bc = T(s2, "g3bc", [128, D]); yo = T(s2, "yo", [128, D]); small = T(s2, "smallD2", [128, 4])
                        p.dma("sp", g3bc[:], g3_d, writes=["g3bc"])
                        for ti in range(8):
                            p.op("act", lambda g, ti=ti: g.activation(out=yo[:], in_=acc[:, ti, :], func=AF.Square, accum_out=small[:, 0:1]), reads=["acc%d" % ti], writes=["yo", "smD"])
                            p.op("act", lambda g: g.activation(out=small[:, 1:2], in_=small[:, 0:1], func=AF.Sqrt, scale=1.0 / D, bias=EPS), reads=["smD"], writes=["smD"])
                            p.op("dve", lambda g: g.reciprocal(out=small[:, 2:3], in_=small[:, 1:2]), reads=["smD"], writes=["smD"])
                            p.op("dve", lambda g, ti=ti: g.scalar_tensor_tensor(out=yo[:], in0=acc[:, ti, :], scalar=small[:, 2:3], in1=g3bc[:], op0=ALU.mult, op1=ALU.mult),
                                 reads=["acc%d" % ti, "smD", "g3bc", "yo"], writes=["yo"])
                            p.dma("sp", out_d[ti * 128:(ti + 1) * 128, :], yo[:], reads=["yo"])
                        p.barrier()
                        p.flush()
            p.barrier()
            p.flush()
    return nc


def _consts():
    ident = np.eye(128, dtype=np.float32)
    rmat = np.zeros((128, 128), np.float32)
    for i in range(16):
        rmat[i + 16, i] = -1.0
        rmat[i, i + 16] = 1.0
    inv_freq = 1.0 / (500000.0 ** (np.arange(0, 32, 2, dtype=np.float32) / 32.0))
    invf2 = np.zeros((128, 1), np.float32)
    invf2[:32, 0] = np.tile(inv_freq, 2) / (2.0 * np.pi)
    iota128 = np.broadcast_to(np.arange(128, dtype=np.float32)[None, :], (128, 128)).copy()
    return ident, rmat, invf2, iota128


def _masks(j):
    past = np.full((128, 8, 16), NEG, np.float32)
    own = np.zeros((128, 8, 16), np.float32)
    for ti in range(8):
        qb = 4 * j + ti // 2
        for rb in range(16):
            tb = (4 * j + rb) % 16
            if tb < qb:
                past[:, ti, rb] = 0.0
            if tb == qb:
                own[:, ti, rb] = 1.0
    return past, own


def make_in_maps(x, positions, attn_norm_g, w_in, gate_bias, conv_w, w_branch_conv, w_branch_attn,
                 w_out, ffn_norm_g, w_peer_query, peer_sub_keys, peer_u, peer_v, final_norm_g):
    f = lambda a: np.ascontiguousarray(np.asarray(a), dtype=np.float32)
    x = f(x)
    positions = np.asarray(positions).astype(np.int32)
    ident, rmat, invf2, iota128 = _consts()
    bc = lambda v: np.ascontiguousarray(np.broadcast_to(f(v).reshape(1, D), (128, D)))
    shared = dict(
        ident=ident, rmat=rmat, invf2=invf2, iota128=iota128,
        g1bc=bc(attn_norm_g[0]), g2bc=bc(ffn_norm_g[0]), g3bc=bc(final_norm_g),
        gbias=np.ascontiguousarray(f(gate_bias[0]).reshape(32, 128).T),
        convw=np.ascontiguousarray(f(conv_w[0]).reshape(3, 8, 128).transpose(2, 1, 0)),
        skT=np.ascontiguousarray(f(peer_sub_keys[0]).reshape(16, 128, 128).transpose(2, 0, 1)),
        w_qkv_l=np.ascontiguousarray(f(w_in[0])[:, 3072:6144].reshape(16, 128, 6, 512).transpose(2, 1, 0, 3)),
        w_cv_l=np.ascontiguousarray(f(w_in[0])[:, 0:3072].reshape(16, 128, 3, 8, 128).transpose(3, 1, 0, 2, 4)),
        w_gt_l=np.ascontiguousarray(f(w_in[0])[:, 6144:10240].reshape(16, 128, 2, 16, 128).transpose(3, 1, 0, 2, 4)),
        w_br_l=np.ascontiguousarray(np.stack([f(w_branch_conv[0]), f(w_branch_attn[0])]).reshape(2, 8, 128, 16, 128).transpose(3, 2, 0, 1, 4)),
        w_out_l=np.ascontiguousarray(f(w_out[0]).reshape(16, 128, 8, 256).transpose(2, 1, 0, 3)),
        w_pq_l=np.ascontiguousarray(f(w_peer_query[0]).reshape(16, 128, 16, 128).transpose(2, 1, 0, 3)),
        u_l=np.ascontiguousarray(f(peer_u[0]).reshape(128, 128, 16, 128).transpose(1, 3, 2, 0)),
        v_l=np.ascontiguousarray(f(peer_v[0]).reshape(128, 128, D).transpose(1, 0, 2)),
    )
    in_maps = []
    for c in range(8):
        b, j = c // 4, c % 4
        m = dict(shared)
        m["xrot"] = np.ascontiguousarray(np.roll(x[b], -j * 1024, axis=0))
        if j == 0:
            m["xhalo"] = np.zeros((128, D), np.float32)
        else:
            m["xhalo"] = np.ascontiguousarray(x[b, j * 1024 - 128:j * 1024])
        prot = np.roll(positions[b], -j * 1024)
        m["posb"] = np.ascontiguousarray(np.broadcast_to(prot[None, :], (128, SEQ))).astype(np.int32)
        past, own = _masks(j)
        m["pastmask"] = past
        m["ownmask"] = own
        in_maps.append(m)
    return in_maps


def kernel(**inputs):
    in_maps = make_in_maps(**inputs)
    nc = build_nc()
    res = run_bass_kernel_spmd(nc, in_maps, core_ids=list(range(8)))
    out = np.zeros((2, SEQ, D), np.float32)
    for c in range(8):
        b, j = c // 4, c % 4
        out[b, j * 1024:(j + 1) * 1024] = res.results[c]["out"]
    return out
```
